# Optimizing a Trainium2 kernel written in Bass

```python
import math
import jax, jax.numpy as jnp
from jax import lax
import numpy as np

D_MODEL = 2048
BATCH = 4
SEQ = 2048
DEPTH = 1

CHUNK = 64
RMS_EPS = 1e-5
RWKV_DIM = D_MODEL // 2
RWKV_HEAD = 64
RWKV_HEADS = RWKV_DIM // RWKV_HEAD
DECAY_LORA = max(32, int(round(1.8 * RWKV_DIM ** 0.5 / 32)) * 32)
A_LORA = max(32, int(round(1.8 * RWKV_DIM ** 0.5 / 32)) * 32)
G_LORA = max(32, int(round(0.6 * RWKV_DIM ** 0.8 / 32)) * 32)
GN_EPS = 64e-5
MLSTM_DIM = D_MODEL - RWKV_DIM
MLSTM_HEADS = 4
MLSTM_HEAD = MLSTM_DIM // MLSTM_HEADS
CONV_W = 4
HEAD_NORM_EPS = 1e-6
N_EXPERTS = 32
TOP_K = 4
D_FF_EXPERT = D_MODEL
SWIGLU_LIMIT = 7.0
SWIGLU_ALPHA = 1.702
MOE_BLOCK = 256
RWKV_SIZES = (RWKV_DIM, DECAY_LORA, RWKV_DIM, RWKV_DIM, A_LORA, G_LORA)
MLSTM_SIZES = (MLSTM_DIM, MLSTM_DIM, MLSTM_DIM, MLSTM_DIM, MLSTM_HEADS, MLSTM_HEADS)
RWKV_COLS = sum(RWKV_SIZES)
MLSTM_COLS = sum(MLSTM_SIZES)
IN_COLS = RWKV_COLS + MLSTM_COLS

kernel_name = "hybrid_rwkv7_mlstm_moe_adaln_block"


def split_cols(a, sizes):
    offs = np.cumsum(sizes)[:-1].tolist()
    return jnp.split(a, offs, axis=-1)


def rms_norm(x, g):
    x32 = x.astype(jnp.float32)
    y = x32 * lax.rsqrt(jnp.mean(x32 * x32, axis=-1, keepdims=True) + RMS_EPS)
    return y.astype(x.dtype) * g


def token_shift(p):
    return jnp.pad(p[:, :-1], ((0, 0), (1, 0), (0, 0)))


def causal_dwconv(p, w, b):
    T = p.shape[1]
    pp = jnp.pad(p, ((0, 0), (CONV_W - 1, 0), (0, 0)))
    out = b
    for j in range(CONV_W):
        out = out + pp[:, j:j + T] * w[j]
    return out


def rwkv7_recurrence(r, w, k, v, a, b):
    Bsz, T, H, N = r.shape
    xs = tuple(jnp.moveaxis(t, 1, 0) for t in (r, w, k, v, a, b))

    def step(S, inp):
        r_t, w_t, k_t, v_t, a_t, b_t = inp
        Sa = jnp.einsum('bhvk,bhk->bhv', S, a_t)
        S = S * w_t[:, :, None, :] + Sa[..., None] * b_t[:, :, None, :] + v_t[..., None] * k_t[:, :, None, :]
        y = jnp.einsum('bhvk,bhk->bhv', S, r_t)
        return S, y

    S0 = jnp.zeros((Bsz, H, N, N), jnp.float32)
    _, ys = lax.scan(step, S0, xs)
    return jnp.moveaxis(ys, 0, 1)


def rwkv7_mixer(p, mu, w0, w2, a0, a2, g2, k_k, k_a, r_k, ln_w, ln_b):
    Bsz, T, _ = p.shape
    f32 = jnp.float32
    p = p + (token_shift(p) - p) * mu
    r, wl, k, v, al, gl = split_cols(p, RWKV_SIZES)
    w = -jax.nn.softplus(-(w0 + jnp.tanh(wl) @ w2)) - 0.5
    decay = jnp.exp(-jnp.exp(w.astype(f32)))
    a = jax.nn.sigmoid(a0 + al @ a2)
    g = jax.nn.sigmoid(gl) @ g2
    hs = (Bsz, T, RWKV_HEADS, RWKV_HEAD)
    kk = (k * k_k).astype(f32).reshape(hs)
    kk = kk / jnp.maximum(jnp.linalg.norm(kk, axis=-1, keepdims=True), 1e-12)
    k = k * (1.0 + (a - 1.0) * k_a)
    a_h = a.astype(f32).reshape(hs)
    r_h = r.astype(f32).reshape(hs)
    k_h = k.astype(f32).reshape(hs)
    v_h = v.astype(f32).reshape(hs)
    y = rwkv7_recurrence(r_h, decay.reshape(hs), k_h, v_h, -kk, kk * a_h)
    mean = jnp.mean(y, axis=-1, keepdims=True)
    var = jnp.mean(jnp.square(y - mean), axis=-1, keepdims=True)
    y = (y - mean) * lax.rsqrt(var + GN_EPS)
    y = y.reshape(Bsz, T, RWKV_DIM) * ln_w + ln_b
    bonus = jnp.sum(r_h * k_h * r_k.astype(f32), axis=-1, keepdims=True) * v_h
    y = y + bonus.reshape(Bsz, T, RWKV_DIM)
    return y.astype(p.dtype) * g


def mlstm_chunkwise(q, k, v, log_i, log_f):
    Bsz, T, H, DK = q.shape
    DV = v.shape[-1]
    L = CHUNK
    NC = T // L
    f32 = jnp.float32

    def chunks(t):
        return t.astype(f32).reshape(Bsz, NC, L, H, t.shape[-1]).transpose(1, 0, 3, 2, 4)

    def gchunks(t):
        return t.astype(f32).reshape(Bsz, NC, L, H).transpose(1, 0, 3, 2)

    causal = jnp.tril(jnp.ones((L, L), dtype=bool))

    def step(carry, inp):
        C, n, m = carry
        qc, kc, vc, li, lf = inp
        b = jnp.cumsum(lf, axis=-1)
        a_inter = b + m[..., None]
        Dm = b[..., :, None] - b[..., None, :] + li[..., None, :]
        Dm = jnp.where(causal, Dm, -jnp.inf)
        m_t = jnp.maximum(a_inter, jnp.max(Dm, axis=-1))
        w_inter = jnp.exp(a_inter - m_t)
        W = jnp.exp(Dm - m_t[..., None])
        S = jnp.einsum('bhtd,bhsd->bhts', qc, kc) * W
        num = w_inter[..., None] * jnp.einsum('bhvd,bhtd->bhtv', C, qc) + jnp.einsum('bhts,bhsv->bhtv', S, vc)
        den = w_inter * jnp.einsum('bhd,bhtd->bht', n, qc) + jnp.sum(S, axis=-1)
        h = num / jnp.maximum(jnp.abs(den), jnp.exp(-m_t))[..., None]
        m_new = m_t[..., -1]
        g_state = jnp.exp(b[..., -1] + m - m_new)
        w_s = jnp.exp(b[..., -1:] - b + li - m_new[..., None])
        C_new = g_state[..., None, None] * C + jnp.einsum('bhsv,bhsd->bhvd', vc * w_s[..., None], kc)
        n_new = g_state[..., None] * n + jnp.einsum('bhs,bhsd->bhd', w_s, kc)
        return (C_new, n_new, m_new), h

    init = (jnp.zeros((Bsz, H, DV, DK), f32), jnp.zeros((Bsz, H, DK), f32), jnp.zeros((Bsz, H), f32))
    _, hs = lax.scan(step, init, (chunks(q), chunks(k), chunks(v), gchunks(log_i), gchunks(log_f)))
    return hs.transpose(1, 0, 3, 2, 4).reshape(Bsz, T, H, DV)


def mlstm_mixer(p, conv_w, conv_b, b_i, b_f, norm_g):
    Bsz, T, _ = p.shape
    f32 = jnp.float32
    qk, v, o, gi, gf = split_cols(p, (2 * MLSTM_DIM, MLSTM_DIM, MLSTM_DIM, MLSTM_HEADS, MLSTM_HEADS))
    qk = jax.nn.silu(causal_dwconv(qk, conv_w, conv_b))
    q, k = jnp.split(qk, 2, axis=-1)
    q = q.reshape(Bsz, T, MLSTM_HEADS, MLSTM_HEAD)
    k = k.reshape(Bsz, T, MLSTM_HEADS, MLSTM_HEAD) * (MLSTM_HEAD ** -0.5)
    v = v.reshape(Bsz, T, MLSTM_HEADS, MLSTM_HEAD)
    log_i = (gi + b_i).astype(f32)
    log_f = jax.nn.log_sigmoid((gf + b_f).astype(f32))
    h = mlstm_chunkwise(q, k, v, log_i, log_f)
    h = h * lax.rsqrt(jnp.mean(h * h, axis=-1, keepdims=True) + HEAD_NORM_EPS)
    h = h.reshape(Bsz, T, MLSTM_DIM).astype(p.dtype) * norm_g
    return h * jax.nn.sigmoid(o)


def moe_ffn(h, router_w, router_b, w_gu, b_gu, w_dn, b_dn):
    N, D = h.shape
    F = D_FF_EXPERT
    logits = (h @ router_w + router_b).astype(jnp.float32)
    top_val, top_idx = lax.top_k(logits, TOP_K)
    gates = jax.nn.softmax(top_val, axis=-1).astype(h.dtype)
    NA = N * TOP_K
    flat_e = top_idx.reshape(-1).astype(jnp.int32)
    flat_tok = jnp.arange(NA, dtype=jnp.int32) // TOP_K
    flat_g = gates.reshape(-1)
    order = jnp.argsort(flat_e)
    se, stok, sg = flat_e[order], flat_tok[order], flat_g[order]
    counts = jnp.bincount(flat_e, length=N_EXPERTS).astype(jnp.int32)
    starts = jnp.cumsum(counts) - counts
    padded = (counts + MOE_BLOCK - 1) // MOE_BLOCK * MOE_BLOCK
    pends = jnp.cumsum(padded)
    pstarts = pends - padded
    dest = pstarts[se] + (jnp.arange(NA, dtype=jnp.int32) - starts[se])
    n_blocks = (NA + MOE_BLOCK - 1) // MOE_BLOCK + N_EXPERTS
    R = n_blocks * MOE_BLOCK
    row_tok = jnp.zeros((R,), jnp.int32).at[dest].set(stok)
    row_g = jnp.zeros((R,), h.dtype).at[dest].set(sg)
    block_start = jnp.arange(n_blocks, dtype=jnp.int32) * MOE_BLOCK
    block_e = jnp.minimum(jnp.searchsorted(pends, block_start, side='right'), N_EXPERTS - 1).astype(jnp.int32)

    def expert_block(args):
        tok, e = args
        xb = h[tok]
        gu = xb @ w_gu[e] + b_gu[e]
        gate, up = gu[:, :F], gu[:, F:]
        gate = jnp.minimum(gate, SWIGLU_LIMIT)
        up = jnp.clip(up, -SWIGLU_LIMIT, SWIGLU_LIMIT)
        act = (up + 1.0) * (gate * jax.nn.sigmoid(SWIGLU_ALPHA * gate))
        return act @ w_dn[e] + b_dn[e]

    out = lax.map(expert_block, (row_tok.reshape(n_blocks, MOE_BLOCK), block_e))
    out = out.reshape(R, D) * row_g[:, None]
    return jnp.zeros((N, D), out.dtype).at[row_tok].add(out)


def setup_inputs(seed: int = 0) -> dict:
    key = jax.random.key(seed)
    ks = iter(jax.random.split(key, 48))
    f32 = jnp.float32
    L, D, E, F = DEPTH, D_MODEL, N_EXPERTS, D_FF_EXPERT

    def nrm(shape, s):
        return jax.random.normal(next(ks), shape, f32) * s

    def unif(shape):
        return jax.random.uniform(next(ks), shape, f32)

    return {
        'x': nrm((BATCH, SEQ, D), 1.0),
        'c': nrm((BATCH, D), 1.0),
        'ada_w': nrm((L, D, 6 * D), 0.5 * D ** -0.5),
        'ada_b': nrm((L, 6 * D), 0.02),
        'norm1_g': 1.0 + nrm((L, D), 0.02),
        'w_in': nrm((L, D, IN_COLS), D ** -0.5),
        'rwkv_mu': unif((L, RWKV_COLS)),
        'rwkv_w0': -2.0 + nrm((L, RWKV_DIM), 0.5),
        'rwkv_w2': nrm((L, DECAY_LORA, RWKV_DIM), 0.1),
        'rwkv_a0': nrm((L, RWKV_DIM), 0.1),
        'rwkv_a2': nrm((L, A_LORA, RWKV_DIM), 0.1),
        'rwkv_g2': nrm((L, G_LORA, RWKV_DIM), G_LORA ** -0.5),
        'rwkv_kk': 0.85 + nrm((L, RWKV_DIM), 0.05),
        'rwkv_ka': 1.0 + nrm((L, RWKV_DIM), 0.05),
        'rwkv_rk': nrm((L, RWKV_HEADS, RWKV_HEAD), 0.1),
        'rwkv_ln_w': 1.0 + nrm((L, RWKV_DIM), 0.02),
        'rwkv_ln_b': nrm((L, RWKV_DIM), 0.02),
        'mlstm_conv_w': nrm((L, CONV_W, 2 * MLSTM_DIM), CONV_W ** -0.5),
        'mlstm_conv_b': nrm((L, 2 * MLSTM_DIM), 0.02),
        'mlstm_b_i': -1.0 + nrm((L, MLSTM_HEADS), 0.1),
        'mlstm_b_f': 3.0 + nrm((L, MLSTM_HEADS), 0.5),
        'mlstm_norm_g': 1.0 + nrm((L, MLSTM_DIM), 0.02),
        'w_out': nrm((L, D, D), D ** -0.5),
        'norm2_g': 1.0 + nrm((L, D), 0.02),
        'router_w': nrm((L, D, E), D ** -0.5),
        'router_b': nrm((L, E), 0.01),
        'moe_w_gu': nrm((L, E, D, 2 * F), D ** -0.5),
        'moe_b_gu': nrm((L, E, 2 * F), 0.01),
        'moe_w_dn': nrm((L, E, F, D), F ** -0.5),
        'moe_b_dn': nrm((L, E, D), 0.01),
        'final_g': 1.0 + nrm((D,), 0.02),
    }


def reference(x, c, ada_w, ada_b, norm1_g, w_in, rwkv_mu, rwkv_w0, rwkv_w2, rwkv_a0, rwkv_a2,
              rwkv_g2, rwkv_kk, rwkv_ka, rwkv_rk, rwkv_ln_w, rwkv_ln_b, mlstm_conv_w, mlstm_conv_b,
              mlstm_b_i, mlstm_b_f, mlstm_norm_g, w_out, norm2_g, router_w, router_b,
              moe_w_gu, moe_b_gu, moe_w_dn, moe_b_dn, final_g):
    Bsz, T, D = x.shape
    for l in range(DEPTH):
        mod = (jax.nn.silu(c) @ ada_w[l] + ada_b[l])[:, None, :]
        sh_m, sc_m, gt_m, sh_f, sc_f, gt_f = jnp.split(mod, 6, axis=-1)

        h = rms_norm(x, norm1_g[l]) * (1.0 + sc_m) + sh_m
        proj = h @ w_in[l]
        p_rwkv, p_mlstm = proj[..., :RWKV_COLS], proj[..., RWKV_COLS:]
        y_rwkv = rwkv7_mixer(p_rwkv, rwkv_mu[l], rwkv_w0[l], rwkv_w2[l], rwkv_a0[l], rwkv_a2[l],
                             rwkv_g2[l], rwkv_kk[l], rwkv_ka[l], rwkv_rk[l], rwkv_ln_w[l], rwkv_ln_b[l])
        y_mlstm = mlstm_mixer(p_mlstm, mlstm_conv_w[l], mlstm_conv_b[l], mlstm_b_i[l], mlstm_b_f[l],
                              mlstm_norm_g[l])
        mix = jnp.concatenate([y_rwkv, y_mlstm], axis=-1) @ w_out[l]
        x = x + gt_m * mix

        h = rms_norm(x, norm2_g[l]) * (1.0 + sc_f) + sh_f
        y = moe_ffn(h.reshape(Bsz * T, D), router_w[l], router_b[l], moe_w_gu[l], moe_b_gu[l],
                    moe_w_dn[l], moe_b_dn[l])
        x = x + gt_f * y.reshape(Bsz, T, D)
    return rms_norm(x, final_g)
```

```python
import contextlib
import numpy as np
import concourse.bass as bass
import concourse.mybir as mybir
from concourse.bass_utils import run_bass_kernel_spmd

F32 = mybir.dt.float32
BF16 = mybir.dt.bfloat16
ALU = mybir.AluOpType
AF = mybir.ActivationFunctionType
AX = mybir.AxisListType

SEMCH = 30000
NDMA = 8
NO_SELF_WAIT = False

T = 2048
D = 2048
KC = 16
RMS_EPS = 1e-5
GN_EPS = 64e-5
HEAD_NORM_EPS = 1e-6
NE = 32
FF = 2048
NTOK = 1024


class Prog:
    ENGS = ("pe", "dve", "act", "pool", "sp")

    def __init__(self, nc):
        self.nc = nc
        self.ops = {e: [] for e in self.ENGS}
        self.nsig = {e: 0 for e in self.ENGS}
        self.ndma = {e: 0 for e in self.ENGS}
        self.known = {e: {} for e in self.ENGS}
        self.lastw = {}
        self.readers = {}
        self.pending = {e: False for e in self.ENGS}

    def _need(self, eng, stamp, waits):
        kind, f, s = stamp
        if kind == 'e':
            if f == eng and (eng == "pe" or NO_SELF_WAIT):
                return
            key = ('e', f)
            if self.known[eng].get(key, 0) >= s:
                return
            assert s <= self.nsig[f], f"wait on pending stamp {stamp} from {eng}"
            self.known[eng][key] = s
            waits.append(stamp)
        else:
            slot = s % NDMA
            cnt = s // NDMA + 1
            key = ('d', f, slot)
            if self.known[eng].get(key, 0) >= cnt:
                return
            self.known[eng][key] = cnt
            waits.append(stamp)

    def _deps(self, eng, reads, writes):
        waits = []
        for r in reads:
            w = self.lastw.get(r)
            if w is not None:
                self._need(eng, w, waits)
        for r in writes:
            w = self.lastw.get(r)
            if w is not None:
                self._need(eng, w, waits)
            for st in self.readers.get(r, ()):
                self._need(eng, st, waits)
        return waits

    def _record(self, stamp, reads, writes):
        for r in reads:
            self.readers.setdefault(r, []).append(stamp)
        for r in writes:
            self.lastw[r] = stamp
            self.readers[r] = []

    def op(self, eng, fn, reads=(), writes=(), signal=True):
        waits = self._deps(eng, reads, writes)
        seq = self.nsig[eng] + 1
        if signal:
            self.nsig[eng] = seq
            self.pending[eng] = False
        else:
            self.pending[eng] = True
        self.ops[eng].append((waits, fn, 'op', signal))
        self._record(('e', eng, seq), reads, writes)

    def dma(self, eng, out, in_, reads=(), writes=()):
        waits = self._deps(eng, reads, writes)
        idx = self.ndma[eng]
        self.ndma[eng] = idx + 1
        if idx >= NDMA:
            self._need(eng, ('d', eng, idx - NDMA), waits)
        self.ops[eng].append((waits, (out, in_), 'dma', idx))
        self._record(('d', eng, idx), reads, writes)

    def dma_custom(self, eng, fn, reads=(), writes=()):
        waits = self._deps(eng, reads, writes)
        idx = self.ndma[eng]
        self.ndma[eng] = idx + 1
        if idx >= NDMA:
            self._need(eng, ('d', eng, idx - NDMA), waits)
        self.ops[eng].append((waits, fn, 'dmac', idx))
        self._record(('d', eng, idx), reads, writes)

    def flush(self, eng):
        if self.pending[eng]:
            self.op(eng, None, signal=True)

    def barrier(self):
        for e in self.ENGS:
            self.flush(e)
        snap = dict(self.nsig)
        dsnap = dict(self.ndma)
        for e in self.ENGS:
            waits = []
            for f in self.ENGS:
                if f != e and snap[f] > 0:
                    self._need(e, ('e', f, snap[f]), waits)
                n = dsnap[f]
                for i in range(max(0, n - NDMA), n):
                    self._need(e, ('d', f, i), waits)
            if waits:
                self.ops[e].append((waits, None, 'nop', False))
        self.lastw = {}
        self.readers = {}

    def emit(self):
        nc = self.nc
        with contextlib.ExitStack() as st:
            esem = {}
            for e in self.ENGS:
                n = (self.nsig[e] + SEMCH - 1) // SEMCH
                esem[e] = [st.enter_context(nc.semaphore(f"s_{e}{i}")) for i in range(max(n, 1))]
            dsem = {}
            for e in self.ENGS:
                if self.ndma[e]:
                    dsem[e] = [st.enter_context(nc.semaphore(f"d_{e}{i}")) for i in range(NDMA)]
            block = st.enter_context(nc.Block())

            def run(engname, engh):
                seq = 0
                for waits, fn, kind, extra in self.ops[engname]:
                    for (k, f, s) in waits:
                        if k == 'e':
                            engh.wait_ge(esem[f][(s - 1) // SEMCH], (s - 1) % SEMCH + 1)
                        else:
                            engh.wait_ge(dsem[f][s % NDMA], 16 * (s // NDMA + 1))
                    if kind == 'op':
                        ins = engh.nop() if fn is None else fn(engh)
                        if extra:
                            seq += 1
                            ins.then_inc(esem[engname][(seq - 1) // SEMCH], 1)
                    elif kind == 'dma':
                        out, in_ = fn
                        engh.dma_start(out=out, in_=in_).then_inc(dsem[engname][extra % NDMA], 16)
                    elif kind == 'dmac':
                        fn(engh).then_inc(dsem[engname][extra % NDMA], 16)

            @block.tensor
            def _(e):
                run("pe", e)

            @block.vector
            def _(e):
                run("dve", e)

            @block.scalar
            def _(e):
                run("act", e)

            @block.gpsimd
            def _(e):
                run("pool", e)

            @block.sync
            def _(e):
                run("sp", e)

    def mm(self, out, lhsT, rhs, start, stop, reads, writes, signal=False, tp=None):
        self.op("pe", lambda e: e.matmul(out, lhsT, rhs, start=start, stop=stop, tile_position=tp),
                reads, writes, signal)

    def tr(self, out, in_, ident, reads, writes, signal=False, tp=None):
        self.op("pe", lambda e: e.transpose(out, in_, ident, tile_position=tp), reads, writes, signal)

    def cp(self, eng, out, in_, reads, writes):
        if eng == "act":
            self.op(eng, lambda e: e.copy(out, in_), reads, writes)
        else:
            self.op(eng, lambda e: e.tensor_copy(out, in_), reads, writes)

    def tt(self, eng, out, a, b, op, reads, writes):
        self.op(eng, lambda e: e.tensor_tensor(out, a, b, op), reads, writes)

    def ts(self, eng, out, a, s1, s2, op0, op1, reads, writes):
        if s2 is None:
            self.op(eng, lambda e: e.tensor_scalar(out, a, s1, None, op0), reads, writes)
        else:
            self.op(eng, lambda e: e.tensor_scalar(out, a, s1, s2, op0, op1), reads, writes)

    def stt(self, out, a, s, b, op0, op1, reads, writes):
        self.op("dve", lambda e: e.scalar_tensor_tensor(out, a, s, b, op0, op1), reads, writes)

    def act(self, out, in_, func, reads, writes, bias=0.0, scale=1.0, accum_out=None):
        if accum_out is None:
            self.op("act", lambda e: e.activation(out, in_, func, bias=bias, scale=scale), reads, writes)
        else:
            self.op("act", lambda e: e.activation(out, in_, func, bias=bias, scale=scale, accum_out=accum_out),
                    reads, writes)

    def rsqrt(self, out, in_, scale, bias, reads, writes):
        self.op("act", lambda e: e.activation(out, in_, AF.Sqrt, bias=bias, scale=scale), reads, writes)
        self.op("dve", lambda e: e.reciprocal(out, out), writes, writes)

    def memset(self, eng, ap, val, writes):
        self.op(eng, lambda e: e.memset(ap, val), (), writes)


NWB = 60


def w_in_col_index():
    idx = np.zeros((NWB, 128), np.int64) - 1
    for p in range(8):
        for j, base in enumerate((0, 1088, 2112)):
            cols = np.concatenate([base + 64 * p + np.arange(64), base + 64 * (p + 8) + np.arange(64)])
            idx[3 * p + j] = cols
    idx[24, :64] = 1024 + np.arange(64)
    idx[24, 64:] = 3136 + np.arange(64)
    idx[25] = 3200 + np.arange(128)
    idx[26, :32] = 3328 + np.arange(32)
    for g, base in enumerate((3360, 4384, 5408, 6432)):
        for i in range(8):
            idx[27 + 8 * g + i] = base + 128 * i + np.arange(128)
    idx[59, :8] = 7456 + np.arange(8)
    return idx


NCH = 4
SEG = NCH * 64
NSEG = T // SEG


def rwkv_phase(nc, P, SB, ps, hT, yT, identf, identb, blockones, sel, w_in_blk, din, dbg, dbg_tensor):
    rw_cols_d = din("rw_cols", [128, 8, 10])
    lora_cols_d = din("lora_cols", [128, 3])
    wa2_d = din("wa2", [128, 8, 128])
    g2a_d = din("g2a", [128, 8, 128])
    g2b_d = din("g2b", [32, 8, 128])
    W = SEG
    yrw_d = dbg_tensor("yrw", [8, 128, T]) if "yrw" in dbg else None
    with contextlib.ExitStack() as st:
        lo_a = SB(st, "lo_a", [128, T])
        sg0 = SB(st, "sg0", [128, T])
        sg1 = SB(st, "sg1", [32, T], BF16)
        wa2 = SB(st, "wa2", [128, 8, 128])
        g2a = SB(st, "g2a", [128, 8, 128])
        g2b = SB(st, "g2b", [32, 8, 128], BF16)
        rwc = SB(st, "rwc", [128, 8, 10])
        rwd = SB(st, "rwd", [128, 8, 6])
        lc = SB(st, "lc", [128, 3])
        lcd = SB(st, "lcd", [128, 3])
        P.dma("sp", wa2[:], wa2_d, (), ["wa2"])
        P.dma("sp", g2a[:], g2a_d, (), ["g2a"])
        P.dma("pool", g2b[:], g2b_d, (), ["g2b"])
        P.dma("sp", rwc[:], rw_cols_d, (), ["rwc"])
        P.dma("sp", lc[:], lora_cols_d, (), ["lc"])
        for j in range(3):
            P.ts("dve", rwd[:, :, j], rwc[:, :, j], -1.0, 1.0, ALU.mult, ALU.add, ["rwc"], ["rwd"])
        P.ts("dve", rwd[:, :, 3], rwc[:, :, 3], -1.0, None, ALU.mult, None, ["rwc"], ["rwd"])
        P.ts("dve", rwd[:, :, 4], rwc[:, :, 4], -1.0, None, ALU.mult, None, ["rwc"], ["rwd"])
        P.ts("dve", rwd[:, :, 5], rwc[:, :, 6], -1.0, 1.0, ALU.mult, ALU.add, ["rwc"], ["rwd"])
        P.ts("dve", lcd[:], lc[:], -1.0, 1.0, ALU.mult, ALU.add, ["lc"], ["lcd"])

        with contextlib.ExitStack() as st2:
            wbf = SB(st2, "wbf", [128, KC, 128], BF16)
            raw = SB(st2, "raw", [128, T])
            tmp = SB(st2, "tmp", [128, T])
            for bi, (blk, dest, npart, dn) in enumerate(((24, lo_a, 128, "lo_a"), (25, sg0, 128, "sg0"), (26, sg1, 32, "sg1"))):
                P.dma("pool", wbf[:], w_in_blk[blk], (), ["wbf"])
                for tb in range(4):
                    pb, pn = ps[tb % 2], f"ps{tb % 2}"
                    for kc in range(KC):
                        P.mm(pb[:, :], wbf[:, kc, :], hT[:, kc, tb * 512:(tb + 1) * 512], kc == 0, kc == KC - 1,
                             ["wbf"], [pn], signal=(kc == KC - 1))
                    P.cp("dve" if tb % 2 else "act", raw[:, tb * 512:(tb + 1) * 512], pb[:, :], [pn], ["raw"])
                n = npart
                P.ts("dve", tmp[0:n, :], raw[0:n, :], lcd[0:n, bi:bi + 1], None, ALU.mult, None, ["raw", "lcd"], ["tmp"])
                P.stt(tmp[0:n, 1:T], raw[0:n, 0:T - 1], lc[0:n, bi:bi + 1], tmp[0:n, 1:T], ALU.mult, ALU.add,
                      ["raw", "lc", "tmp"], ["tmp"])
                if blk == 24:
                    P.act(dest[0:64, :], tmp[0:64, :], AF.Tanh, ["tmp"], [dn])
                    P.cp("act", dest[64:128, :], tmp[64:128, :], ["tmp"], [dn])
                else:
                    P.act(dest[0:n, :], tmp[0:n, :], AF.Sigmoid, ["tmp"], [dn])
            if "lora" in dbg:
                P.dma("sp", dbg_tensor("lo_a", [128, T]), lo_a[:], ["lo_a"], ())
                P.dma("sp", dbg_tensor("sg0", [128, T]), sg0[:], ["sg0"], ())
            P.barrier()

        with contextlib.ExitStack() as st3:
            def TL(name, w=W, dt=F32):
                return SB(st3, name, [128, w], dt)
            wps = [SB(st3, f"wp{i}", [128, KC, 3, 128], BF16) for i in range(2)]
            mSU = TL("mSU"); mU = TL("mU"); mSL = TL("mSL"); idr = TL("idr"); ones_t = TL("ones_t", 64)
            praw = [TL(f"praw{j}", W + 1) for j in range(3)]
            sh = [TL(f"sh{j}") for j in range(3)]
            tq = [TL(f"tq{j}") for j in range(8)]
            Bt = TL("Bt", dt=BF16); Kt = TL("Kt", dt=BF16); Bh = TL("Bh", dt=BF16); Kh = TL("Kh", dt=BF16)
            Qm = TL("Qm", dt=BF16); QTm = TL("QTm", dt=BF16); AkT = TL("AkT", dt=BF16); Vb = TL("Vb", dt=BF16)
            DB = {}
            for nm in ("At", "Rt", "Xm", "ArbT", "ArkT", "Vtm", "Bhtm", "Khtm"):
                DB[nm] = [TL(f"{nm}{i}", dt=BF16) for i in range(2)]
            for nm in ("AkV", "bonus", "gfm"):
                DB[nm] = [TL(f"{nm}{i}") for i in range(2)]
            DB["gamL"] = [SB(st3, f"gamL{i}", [128, NCH]) for i in range(2)]
            gy = TL("gy"); gq = TL("gq")
            ST = SB(st3, "ST", [128, 64], BF16)
            Zt = SB(st3, "Zt", [128, 64], BF16)
            Ut = SB(st3, "Ut", [128, 64], BF16)

            for (m, mn, pat, cm, cmp_) in ((mSU, "mSU", [[0, NCH], [1, 64]], -1, ALU.is_gt),
                                          (mU, "mU", [[0, NCH], [1, 64]], -1, ALU.is_ge),
                                          (mSL, "mSL", [[0, NCH], [-1, 64]], 1, ALU.is_gt),
                                          (idr, "idr", [[0, NCH], [1, 64]], -1, ALU.is_equal)):
                P.memset("pool", m[:], 1.0, [mn])
                for hp in range(2):
                    sl = slice(hp * 64, (hp + 1) * 64)
                    P.op("pool", (lambda e, m=m, sl=sl, pat=pat, cm=cm, cmp_=cmp_:
                                  e.affine_select(m[sl, :], m[sl, :], pat, cmp_, 0.0, base=0, channel_multiplier=cm)),
                         [mn], [mn])
            P.memset("pool", ones_t[:], 1.0, ["ones_t"])

            H = [slice(0, 64), slice(64, 128)]
            TP = [(0, 0), (64, 64)]

            def C(c):
                return slice(c * 64, (c + 1) * 64)

            def cmm(bank, lhs, ln, rhs, rn):
                for c in range(NCH):
                    for hp in range(2):
                        P.mm(ps[bank][H[hp], C(c)], lhs[H[hp], C(c)], rhs[H[hp], C(c)], True, True, [ln, rn], [f"ps{bank}"],
                             signal=(c == NCH - 1 and hp == 1), tp=TP[hp])

            def pre_stages(u):
                p, s_ = divmod(u, NSEG)
                par = u % 2
                t0 = s_ * W
                col = lambda j: rwc[:, p, j:j + 1]
                dcol = lambda j: rwd[:, p, j:j + 1]
                wp, wpn = wps[p % 2], f"wp{p % 2}"
                At, Rt, Xm, AkV, ArbT, ArkT = (DB[n][par] for n in ("At", "Rt", "Xm", "AkV", "ArbT", "ArkT"))
                Vtm, Bhtm, Khtm, bonus, gfm, gamL = (DB[n][par] for n in ("Vtm", "Bhtm", "Khtm", "bonus", "gfm", "gamL"))
                nAt, nRt, nXm, nAkV, nArbT, nArkT = (f"{n}{par}" for n in ("At", "Rt", "Xm", "AkV", "ArbT", "ArkT"))
                nVtm, nBhtm, nKhtm, nbonus, ngfm, ngamL = (f"{n}{par}" for n in ("Vtm", "Bhtm", "Khtm", "bonus", "gfm", "gamL"))
                shr, shk, shv = sh
                st_ = []

                def S(f):
                    st_.append(f)
                    return f

                @S
                def _():
                    if s_ == 0:
                        for pp in ([0, 1] if p == 0 else ([p + 1] if p + 1 < 8 else [])):
                            for j in range(3):
                                P.dma("pool", wps[pp % 2][:, :, j, :], w_in_blk[3 * pp + j], (), [f"wp{pp % 2}"])
                        for j in range(3):
                            P.memset("dve", praw[j][:, 0:1], 0.0, [f"praw{j}"])
                    for j in range(3):
                        for kc in range(KC):
                            P.mm(ps[j][:, 0:W], wp[:, kc, j, :], hT[:, kc, t0:t0 + W], kc == 0, kc == KC - 1,
                                 [wpn], [f"ps{j}"], signal=(kc == KC - 1))

                @S
                def _():
                    for j in range(3):
                        P.cp("act", praw[j][:, 1:W + 1], ps[j][:, 0:W], [f"ps{j}"], [f"praw{j}"])
                        P.ts("dve", tq[0][:], praw[j][:, 1:W + 1], dcol(j), None, ALU.mult, None, [f"praw{j}", "rwd"], ["tq0"])
                        P.stt(sh[j][:], praw[j][:, 0:W], col(j), tq[0][:], ALU.mult, ALU.add, [f"praw{j}", "rwc", "tq0"], [f"sh{j}"])
                        P.cp("dve", praw[j][:, 0:1], praw[j][:, W:W + 1], [f"praw{j}"], [f"praw{j}"])
                    P.mm(ps[3][:, 0:W], wa2[0:64, p, :], lo_a[0:64, t0:t0 + W], True, True, ["wa2"], ["ps3"], signal=True)
                    P.mm(ps[0][:, 0:W], wa2[64:128, p, :], lo_a[64:128, t0:t0 + W], True, True, ["wa2"], ["ps0"], signal=True)
                    P.mm(ps[1][:, 0:W], g2a[:, p, :], sg0[:, t0:t0 + W], True, False, ["g2a"], ["ps1"])
                    P.mm(ps[1][:, 0:W], g2b[0:32, p, :], sg1[0:32, t0:t0 + W], False, True, ["g2b"], ["ps1"], signal=True)

                @S
                def _():
                    P.act(tq[1][:], ps[3][:, 0:W], AF.Exp, ["ps3", "rwd"], ["tq1"], bias=dcol(3), scale=-1.0)
                    P.act(tq[1][:], tq[1][:], AF.Ln, ["tq1"], ["tq1"], bias=1.0)
                    P.act(tq[1][:], tq[1][:], AF.Exp, ["tq1"], ["tq1"], bias=-0.5, scale=-1.0)
                    P.ts("dve", tq[3][:], shk[:], col(5), None, ALU.mult, None, ["sh1", "rwc"], ["tq3"])
                    P.tt("dve", tq[4][:], tq[3][:], tq[3][:], ALU.mult, ["tq3"], ["tq4"])
                    P.mm(ps[2][:, 0:W], blockones[:], tq[4][:], True, True, ["blockones", "tq4"], ["ps2"], signal=True)
                    P.ts("dve", tq[1][:], tq[1][:], -1.0, None, ALU.mult, None, ["tq1"], ["tq1"])
                    P.act(tq[2][:], ps[0][:, 0:W], AF.Exp, ["ps0", "rwd"], ["tq2"], bias=dcol(4), scale=-1.0)
                    P.act(tq[2][:], tq[2][:], AF.Ln, ["tq2"], ["tq2"], bias=1.0)
                    P.act(tq[2][:], tq[2][:], AF.Exp, ["tq2"], ["tq2"], scale=-1.0)
                    P.cp("act", gfm[:], ps[1][:, 0:W], ["ps1"], [ngfm])

                @S
                def _():
                    P.ts("dve", tq[4][:], ps[2][:, 0:W], 1e-24, None, ALU.max, None, ["ps2"], ["tq4"])
                    P.act(tq[4][:], tq[4][:], AF.Sqrt, ["tq4"], ["tq4"])
                    P.op("dve", lambda e: e.reciprocal(tq[4][:], tq[4][:]), ["tq4"], ["tq4"])
                    P.tt("dve", tq[3][:], tq[3][:], tq[4][:], ALU.mult, ["tq3", "tq4"], ["tq3"])
                    P.ts("dve", tq[4][:], tq[2][:], col(6), dcol(5), ALU.mult, ALU.add, ["tq2", "rwc", "rwd"], ["tq4"])
                    P.tt("dve", tq[4][:], shk[:], tq[4][:], ALU.mult, ["sh1", "tq4"], ["tq4"])
                    P.tt("dve", tq[2][:], tq[3][:], tq[2][:], ALU.mult, ["tq3", "tq2"], ["tq2"])
                    P.stt(tq[5][:], shr[:], col(7), tq[4][:], ALU.mult, ALU.mult, ["sh0", "rwc", "tq4"], ["tq5"])
                    P.mm(ps[3][:, 0:W], blockones[:], tq[5][:], True, True, ["blockones", "tq5"], ["ps3"], signal=True)
                    P.tt("dve", bonus[:], ps[3][:, 0:W], shv[:], ALU.mult, ["ps3", "sh2"], [nbonus])

                @S
                def _():
                    for c in range(NCH):
                        P.op("dve", (lambda e, c=c: e.tensor_tensor_scan(tq[5][:, C(c)], ones_t[:, 0:64], tq[1][:, C(c)],
                                                                          0.0, ALU.mult, ALU.add)),
                             ["ones_t", "tq1"], ["tq5"])
                    lg3 = tq[5][:].rearrange("p (c t) -> p c t", t=64)
                    P.act(tq[6][:], tq[5][:], AF.Exp, ["tq5"], ["tq6"])
                    P.tt("dve", tq[7][:], tq[5][:], tq[1][:], ALU.subtract, ["tq5", "tq1"], ["tq7"])
                    P.tt("dve", Rt[:], shr[:], tq[6][:], ALU.mult, ["sh0", "tq6"], [nRt])
                    P.act(tq[7][:], tq[7][:], AF.Exp, ["tq7"], ["tq7"])
                    P.act(tq[6][:], tq[5][:], AF.Exp, ["tq5"], ["tq6"], scale=-1.0)
                    P.stt(At[:], tq[3][:], -1.0, tq[7][:], ALU.mult, ALU.mult, ["tq3", "tq7"], [nAt])
                    P.tt("dve", tq[7][:].rearrange("p (c t) -> p c t", t=64), lg3[:, :, 63:64].to_broadcast([128, NCH, 64]),
                         lg3, ALU.subtract, ["tq5"], ["tq7"])
                    P.act(gamL[:], lg3[:, :, 63], AF.Exp, ["tq5"], [ngamL])
                    P.act(tq[7][:], tq[7][:], AF.Exp, ["tq7"], ["tq7"])
                    P.tt("dve", Bt[:], tq[2][:], tq[6][:], ALU.mult, ["tq2", "tq6"], ["Bt"])
                    P.tt("dve", Kt[:], tq[4][:], tq[6][:], ALU.mult, ["tq4", "tq6"], ["Kt"])
                    P.tt("dve", Bh[:], tq[2][:], tq[7][:], ALU.mult, ["tq2", "tq7"], ["Bh"])
                    P.tt("dve", Kh[:], tq[4][:], tq[7][:], ALU.mult, ["tq4", "tq7"], ["Kh"])

                @S
                def _():
                    P.cp("act", Vb[:], shv[:], ["sh2"], ["Vb"])
                    for (src, sn, bank) in ((Bh, "Bh", 0), (Kh, "Kh", 1), (Vb, "Vb", 2)):
                        for c in range(NCH):
                            for hp in range(2):
                                P.mm(ps[bank][H[hp], C(c)], src[H[hp], C(c)], identb[H[hp], H[hp]], True, True, [sn], [f"ps{bank}"],
                                     signal=(c == NCH - 1 and hp == 1), tp=TP[hp])
                    cmm(3, Bt, "Bt", At, nAt)

                @S
                def _():
                    P.cp("act", Bhtm[:], ps[0][:, 0:W], ["ps0"], [nBhtm])
                    P.cp("act", Khtm[:], ps[1][:, 0:W], ["ps1"], [nKhtm])
                    P.cp("act", Vtm[:], ps[2][:, 0:W], ["ps2"], [nVtm])
                    P.tt("dve", Qm[:], ps[3][:, 0:W], mSU[:], ALU.mult, ["ps3", "mSU"], ["Qm"])
                    P.tt("dve", Xm[:], Qm[:], idr[:], ALU.add, ["Qm", "idr"], [nXm])
                    cmm(0, At, nAt, Bt, "Bt")
                    cmm(1, Kt, "Kt", At, nAt)
                    cmm(2, Bt, "Bt", Rt, nRt)
                    cmm(3, Kt, "Kt", Rt, nRt)

                @S
                def _():
                    P.tt("dve", QTm[:], ps[0][:, 0:W], mSL[:], ALU.mult, ["ps0", "mSL"], ["QTm"])
                    P.tt("dve", AkT[:], ps[1][:, 0:W], mSU[:], ALU.mult, ["ps1", "mSU"], ["AkT"])
                    P.tt("dve", ArbT[:], ps[2][:, 0:W], mU[:], ALU.mult, ["ps2", "mU"], [nArbT])
                    P.tt("dve", ArkT[:], ps[3][:, 0:W], mU[:], ALU.mult, ["ps3", "mU"], [nArkT])
                    cmm(0, AkT, "AkT", Vtm, nVtm)
                    cmm(1, QTm, "QTm", Qm, "Qm")
                    cmm(2, Qm, "Qm", QTm, "QTm")

                @S
                def _():
                    P.cp("act", AkV[:], ps[0][:, 0:W], ["ps0"], [nAkV])
                    P.cp("act", Qm[:], ps[1][:, 0:W], ["ps1"], ["Qm"])
                    P.cp("act", QTm[:], ps[2][:, 0:W], ["ps2"], ["QTm"])

                for it in range(5):
                    def mmstep(it=it):
                        cmm(3, QTm, "QTm", Xm, nXm)
                        if it < 4:
                            cmm(1, QTm, "QTm", Qm, "Qm")
                            cmm(2, Qm, "Qm", QTm, "QTm")
                    def evstep(it=it):
                        P.tt("dve", Xm[:], Xm[:], ps[3][:, 0:W], ALU.add, [nXm, "ps3"], [nXm])
                        if it < 4:
                            P.cp("act", Qm[:], ps[1][:, 0:W], ["ps1"], ["Qm"])
                            P.cp("act", QTm[:], ps[2][:, 0:W], ["ps2"], ["QTm"])
                    st_.append(mmstep)
                    st_.append(evstep)
                return st_

            def chain_hops(u):
                p, s_ = divmod(u, NSEG)
                par = u % 2
                t0 = s_ * W
                col = lambda j: rwc[:, p, j:j + 1]
                At, Rt, Xm, AkV, ArbT, ArkT = (DB[n][par] for n in ("At", "Rt", "Xm", "AkV", "ArbT", "ArkT"))
                Vtm, Bhtm, Khtm, bonus, gfm, gamL = (DB[n][par] for n in ("Vtm", "Bhtm", "Khtm", "bonus", "gfm", "gamL"))
                nAt, nRt, nXm, nAkV, nArbT, nArkT = (f"{n}{par}" for n in ("At", "Rt", "Xm", "AkV", "ArbT", "ArkT"))
                nVtm, nBhtm, nKhtm, nbonus, ngfm, ngamL = (f"{n}{par}" for n in ("Vtm", "Bhtm", "Khtm", "bonus", "gfm", "gamL"))
                hops = []
                for c in range(NCH):
                    def h1(c=c):
                        if s_ == 0 and c == 0:
                            P.memset("dve", ST[:], 0.0, ["ST"])
                        for hp in range(2):
                            P.mm(ps[4][H[hp], C(c)], At[H[hp], C(c)], ST[H[hp], :], True, True, [nAt, "ST"], ["ps4"],
                                 signal=(hp == 1), tp=TP[hp])
                    def h2(c=c):
                        P.tt("dve", Zt[:], ps[4][:, C(c)], AkV[:, C(c)], ALU.add, ["ps4", nAkV], ["Zt"])
                        for hp in range(2):
                            P.mm(ps[5][H[hp], C(c)], Xm[H[hp], C(c)], Zt[H[hp], :], True, True, [nXm, "Zt"], ["ps5"],
                                 signal=(hp == 1), tp=TP[hp])
                    def h3(c=c):
                        P.cp("act", Ut[:], ps[5][:, C(c)], ["ps5"], ["Ut"])
                        for hp in range(2):
                            P.mm(ps[7][H[hp], C(c)], ST[H[hp], :], Rt[H[hp], C(c)], True, False, ["ST", nRt], ["ps7"], tp=TP[hp])
                            P.mm(ps[7][H[hp], C(c)], Ut[H[hp], :], ArbT[H[hp], C(c)], False, False, ["Ut", nArbT], ["ps7"], tp=TP[hp])
                            P.mm(ps[7][H[hp], C(c)], Vtm[H[hp], C(c)], ArkT[H[hp], C(c)], False, True, [nVtm, nArkT], ["ps7"], tp=TP[hp])
                        for hp in range(2):
                            P.mm(ps[6][H[hp], C(c)], Bhtm[H[hp], C(c)], Ut[H[hp], :], True, False, [nBhtm, "Ut"], ["ps6"], tp=TP[hp])
                            P.mm(ps[6][H[hp], C(c)], Khtm[H[hp], C(c)], Vtm[H[hp], C(c)], False, True, [nKhtm, nVtm], ["ps6"],
                                 signal=(hp == 1), tp=TP[hp])
                    def h4(c=c):
                        P.stt(ST[:], ST[:], gamL[:, c:c + 1], ps[6][:, C(c)], ALU.mult, ALU.add, ["ST", ngamL, "ps6"], ["ST"])
                    hops += [h1, h2, h3, h4]

                def g1():
                    P.cp("act", gy[:], ps[7][:, 0:W], ["ps7"], ["gy"])
                    P.mm(ps[4][:, 0:W], blockones[:], gy[:], True, True, ["blockones", "gy"], ["ps4"], signal=True)
                def g2():
                    P.stt(gy[:], ps[4][:, 0:W], -1.0 / 64, gy[:], ALU.mult, ALU.add, ["ps4", "gy"], ["gy"])
                    P.tt("dve", gq[:], gy[:], gy[:], ALU.mult, ["gy"], ["gq"])
                    P.mm(ps[5][:, 0:W], blockones[:], gq[:], True, True, ["blockones", "gq"], ["ps5"], signal=True)
                def g3():
                    P.act(gq[:], ps[5][:, 0:W], AF.Ln, ["ps5"], ["gq"], bias=GN_EPS, scale=1.0 / 64)
                    P.act(gq[:], gq[:], AF.Exp, ["gq"], ["gq"], scale=-0.5)
                    P.tt("dve", gy[:], gy[:], gq[:], ALU.mult, ["gy", "gq"], ["gy"])
                    P.ts("dve", gy[:], gy[:], col(8), col(9), ALU.mult, ALU.add, ["gy", "rwc"], ["gy"])
                    P.tt("dve", gy[:], gy[:], bonus[:], ALU.add, ["gy", nbonus], ["gy"])
                    P.tt("dve", gy[:], gy[:], gfm[:], ALU.mult, ["gy", ngfm], ["gy"])
                    if yrw_d is not None:
                        P.dma("sp", yrw_d[p][:, t0:t0 + W], gy[:], ["gy"], ())
                    half = t0 // NTOK
                    c0 = t0 % NTOK
                    if half == 0:
                        P.ts("dve", yT[:, p, c0:c0 + W], gy[:], sel[:, 0:1], None, ALU.mult, None, ["gy", "sel"], ["yT"])
                    else:
                        P.stt(yT[:, p, c0:c0 + W], gy[:], sel[:, 1:2], yT[:, p, c0:c0 + W], ALU.mult, ALU.add,
                              ["gy", "sel", "yT"], ["yT"])
                hops += [g1, g2, g3]
                return hops

            NU = 8 * NSEG
            for u in range(NU + 1):
                A_ = pre_stages(u) if u < NU else []
                B_ = chain_hops(u - 1) if u >= 1 else []
                for k in range(max(len(A_), len(B_))):
                    if k < len(A_):
                        A_[k]()
                    if k < len(B_):
                        B_[k]()
            P.barrier()
    P.barrier()


def mlstm_phase(nc, P, SB, ps, hT, yT, identf, identb, sel, w_in_blk, din, dbg, dbg_tensor):
    ml_cols_d = din("ml_cols", [128, 16, 5])
    ml_gb_d = din("ml_gb", [4, 2])
    ml_ng_d = din("ml_ng", [1, 1024])
    yml_d = dbg_tensor("yml", [T, 1024]) if "yml" in dbg else None
    H = [slice(0, 64), slice(64, 128)]
    with contextlib.ExitStack() as st:
        mlc = SB(st, "mlc", [128, 16, 5])
        gb = SB(st, "gb", [4, 2])
        ngb = SB(st, "ngb", [128, 1024])
        colsT = SB(st, "colsT", [128, 16, 36])
        gB = SB(st, "gB", [128, 4, 32])
        mUm = SB(st, "mUm", [128, 64])
        wbfs = [SB(st, f"wbf2_{i}", [128, KC, 128], BF16) for i in range(2)]
        wbf = wbfs[0]
        P.dma("sp", mlc[:], ml_cols_d, (), ["mlc"])
        P.dma("sp", gb[:], ml_gb_d, (), ["gb"])
        P.dma("sp", ngb[:], ml_ng_d[0:1, :].partition_broadcast(128), (), ["ngb"])
        P.memset("pool", mUm[:], 1.0, ["mUm"])
        for hp in range(2):
            P.op("pool", (lambda e, hp=hp: e.affine_select(mUm[H[hp], :], mUm[H[hp], :], [[1, 64]], ALU.is_ge, 0.0,
                                                           base=0, channel_multiplier=-1)), ["mUm"], ["mUm"])
        with contextlib.ExitStack() as st2:
            A = SB(st2, "gA", [4, T]); B = SB(st2, "gB_", [4, T]); Fb = SB(st2, "gF", [4, T])
            Rrow = SB(st2, "Rrow", [128, T])
            onesr = SB(st2, "onesr", [4, T])
            Me = SB(st2, "Me", [4, 32]); gr = SB(st2, "gr", [4, 32]); nbf = SB(st2, "nbf", [4, 1])
            selrows = SB(st2, "selrows", [4, 4, 128])
            P.memset("pool", Rrow[:], 0.0, ["Rrow"])
            P.memset("pool", onesr[:], 1.0, ["onesr"])
            P.memset("pool", Me[:], 0.0, ["Me"])
            P.ts("dve", nbf[:], gb[:, 1:2], -1.0, None, ALU.mult, None, ["gb"], ["nbf"])
            for h in range(4):
                P.cp("dve", selrows[0:4, h, :], identf[0:4, h:h + 1].to_broadcast([4, 128]), ["identf"], ["selrows"])
            P.dma("pool", wbf[:], w_in_blk[59], (), ["wbf2"])
            for tb in range(4):
                for kc in range(KC):
                    P.mm(ps[0][0:4, :], wbf[:, kc, 0:4], hT[:, kc, tb * 512:(tb + 1) * 512], kc == 0, kc == KC - 1, ["wbf2"], ["ps0"])
                for kc in range(KC):
                    P.mm(ps[1][0:4, :], wbf[:, kc, 4:8], hT[:, kc, tb * 512:(tb + 1) * 512], kc == 0, kc == KC - 1, ["wbf2"], ["ps1"],
                         signal=(kc == KC - 1))
                sl = slice(tb * 512, (tb + 1) * 512)
                P.ts("dve", A[:, sl], ps[0][0:4, :], gb[:, 0:1], None, ALU.add, None, ["ps0", "gb"], ["gA"])
                P.act(B[:, sl], ps[1][0:4, :], AF.Exp, ["ps1", "nbf"], ["gB_"], bias=nbf[:, 0:1], scale=-1.0)
            P.act(B[:], B[:], AF.Ln, ["gB_"], ["gB_"], bias=1.0)
            P.ts("dve", B[:], B[:], -1.0, None, ALU.mult, None, ["gB_"], ["gB_"])
            P.op("dve", lambda e: e.tensor_tensor_scan(Fb[:], onesr[:], B[:], 0.0, ALU.mult, ALU.add), ["onesr", "gB_"], ["gF"])
            P.tt("dve", A[:], A[:], Fb[:], ALU.subtract, ["gA", "gF"], ["gA"])
            P.op("dve", lambda e: e.tensor_tensor_scan(B[:], onesr[:], A[:], 0.0, ALU.mult, ALU.max), ["onesr", "gA"], ["gB_"])
            M3 = B[:].rearrange("p (c t) -> p c t", t=64)
            A3 = A[:].rearrange("p (c t) -> p c t", t=64)
            F3 = Fb[:].rearrange("p (c t) -> p c t", t=64)
            P.cp("dve", Me[:, 1:32], M3[:, 0:31, 63], ["gB_"], ["Me"])
            P.tt("dve", gr[:], Me[:], M3[:, :, 63], ALU.subtract, ["Me", "gB_"], ["gr"])
            P.act(gr[:], gr[:], AF.Exp, ["gr"], ["gr"])
            Meb = Me[:].unsqueeze(2).to_broadcast([4, 32, 64])
            R0 = Rrow[0:4, :].rearrange("p (c t) -> p c t", t=64)
            R1 = Rrow[32:36, :].rearrange("p (c t) -> p c t", t=64)
            P.tt("dve", R0, A3, Meb, ALU.subtract, ["gA", "Me"], ["Rrow"])
            P.act(Rrow[0:4, :], Rrow[0:4, :], AF.Exp, ["Rrow"], ["Rrow"])
            P.tt("dve", R1, F3, Meb, ALU.add, ["gF", "Me"], ["Rrow"])
            P.act(Rrow[32:36, :], Rrow[32:36, :], AF.Exp, ["Rrow"], ["Rrow"], scale=-1.0)
            for jj in range(16):
                pb, pn = ps[2 + jj % 2], f"ps{2 + jj % 2}"
                P.tr(pb[:, 0:128], Rrow[:, jj * 128:(jj + 1) * 128], identf[:], ["Rrow", "identf"], [pn], signal=True)
                P.cp("act" if jj % 2 else "dve", colsT[:, jj, :], pb[:, 0:36], [pn], ["colsT"])
            for h in range(4):
                P.mm(ps[4][:, h * 32:(h + 1) * 32], selrows[0:4, h, :], gr[0:4, :], True, True, ["selrows", "gr"], ["ps4"], signal=(h == 3))
            P.cp("dve", gB[:].rearrange("p h c -> p (h c)"), ps[4][:, 0:128], ["ps4"], ["gB"])
            P.barrier()

        with contextlib.ExitStack() as st3:
            qfm = SB(st3, "qfm", [128, 2, T], BF16); kfm = SB(st3, "kfm", [128, 2, T], BF16)
            cacc = SB(st3, "cacc", [128, T])
            rawc = SB(st3, "rawc", [128, T + 3])
            wv = SB(st3, "wv", [128, KC, 256], BF16); wo = SB(st3, "wo", [128, KC, 256], BF16)
            CT = SB(st3, "CT", [128, 2, 257], BF16)
            ktms = [SB(st3, f"ktm{i}", [128, 256], BF16) for i in range(2)]
            vaugs = [SB(st3, f"vaug{i}", [128, 257]) for i in range(2)]
            sgos = [SB(st3, f"sgo{i}", [128, 256]) for i in range(2)]
            STss = [SB(st3, f"STs{i}", [128, 64], BF16) for i in range(2)]
            vus = [SB(st3, f"vu{i}", [128, 257], BF16) for i in range(2)]
            hh = SB(st3, "hh", [128, 256]); yv = SB(st3, "yv", [128, 256]); junk = SB(st3, "junk2", [128, 256])
            rd = SB(st3, "rd", [128, 1]); ssq = SB(st3, "ssq", [128, 1]); rn = SB(st3, "rn", [128, 1])
            P.memset("pool", rawc[:, 0:3], 0.0, ["rawc"])
            for i in range(2):
                P.memset("pool", vaugs[i][:, 256:257], 1.0, [f"vaug{i}"])
            for h in range(4):
                for which, dst, dn in ((0, qfm, "qfm"), (1, kfm, "kfm")):
                    for blk in range(2):
                        bi = 27 + 8 * which + 2 * h + blk
                        ci = 8 * which + 2 * h + blk
                        wbf, wbn = wbfs[ci % 2], f"wbf2_{ci % 2}"
                        P.dma("pool", wbf[:], w_in_blk[bi], (), [wbn])
                        for tb in range(4):
                            pb, pn = ps[tb % 2], f"ps{tb % 2}"
                            for kc in range(KC):
                                P.mm(pb[:, :], wbf[:, kc, :], hT[:, kc, tb * 512:(tb + 1) * 512], kc == 0, kc == KC - 1, [wbn], [pn],
                                     signal=(kc == KC - 1))
                            P.cp("dve" if tb % 2 else "act", rawc[:, 3 + tb * 512:3 + (tb + 1) * 512], pb[:, :], [pn], ["rawc"])
                        d_ = dst[:, blk, :]
                        P.ts("dve", cacc[:], rawc[:, 3:3 + T], mlc[:, ci, 3:4], mlc[:, ci, 4:5], ALU.mult, ALU.add, ["rawc", "mlc"], ["cacc"])
                        for j in range(3):
                            P.stt(cacc[:], rawc[:, j:j + T], mlc[:, ci, j:j + 1], cacc[:], ALU.mult, ALU.add, ["rawc", "mlc", "cacc"], ["cacc"])
                        P.act(d_, cacc[:], AF.Silu, ["cacc"], [dn])
                        if which == 1:
                            P.ts("dve", d_, d_, 0.0625, None, ALU.mult, None, [dn], [dn])
                for which, dst, dn in ((2, wv, "wv"), (3, wo, "wo")):
                    for blk in range(2):
                        P.dma("pool", dst[:, :, blk * 128:(blk + 1) * 128], w_in_blk[27 + 8 * which + 2 * h + blk], (), [dn])
                P.memset("pool", CT[:], 0.0, ["CT"])

                def A_stages(jj, h=h):
                    q2 = jj % 2
                    vaug, sgo, ktm, STs, vu = vaugs[q2], sgos[q2], ktms[q2], STss[q2], vus[q2]
                    nv, nsg, nkt, nst, nvu = f"vaug{q2}", f"sgo{q2}", f"ktm{q2}", f"STs{q2}", f"vu{q2}"
                    tk = slice(jj * 128, (jj + 1) * 128)
                    st_ = []

                    def s1():
                        for kc in range(KC):
                            P.mm(ps[0][:, 0:256], hT[:, kc, tk], wv[:, kc, :], kc == 0, kc == KC - 1, ["wv"], ["ps0"], signal=(kc == KC - 1))
                        for kc in range(KC):
                            P.mm(ps[1][:, 0:256], hT[:, kc, tk], wo[:, kc, :], kc == 0, kc == KC - 1, ["wo"], ["ps1"], signal=(kc == KC - 1))
                        ps7b = ps[7][:].bitcast(BF16)
                        for blk in range(2):
                            P.tr(ps7b[:, blk * 128:(blk + 1) * 128], kfm[:, blk, tk], identb[:], ["kfm", "identb"], ["ps7"], signal=(blk == 1))
                        for par in range(2):
                            c = 2 * jj + par
                            tks = slice(c * 64, (c + 1) * 64)
                            for blk in range(2):
                                P.mm(ps[2][H[par], 0:64], kfm[:, blk, tks], qfm[:, blk, tks], blk == 0, blk == 1, ["kfm", "qfm"], ["ps2"],
                                     signal=(blk == 1), tp=(0, 64 * par))

                    def s2():
                        P.cp("dve", vaug[:, 0:256], ps[0][:, 0:256], ["ps0"], [nv])
                        P.act(sgo[:], ps[1][:, 0:256], AF.Sigmoid, ["ps1"], [nsg])
                        P.cp("act", ktm[:], ps[7][:].bitcast(BF16)[:, 0:256], ["ps7"], [nkt])
                        P.tt("dve", STs[:], ps[2][:, 0:64], mUm[:], ALU.mult, ["ps2", "mUm"], [nst])
                        for par in range(2):
                            Hs = H[par]
                            P.ts("dve", vu[Hs, :], vaug[Hs, :], colsT[Hs, jj, h:h + 1], None, ALU.mult, None, [nv, "colsT"], [nvu])
                    return [s1, s2]

                def B_hops(jj, h=h):
                    q2 = jj % 2
                    vaug, sgo, ktm, STs, vu = vaugs[q2], sgos[q2], ktms[q2], STss[q2], vus[q2]
                    nv, nsg, nkt, nst, nvu = f"vaug{q2}", f"sgo{q2}", f"ktm{q2}", f"STs{q2}", f"vu{q2}"
                    tk = slice(jj * 128, (jj + 1) * 128)
                    hops = []
                    for par in range(2):
                        c = 2 * jj + par
                        Hs = H[par]
                        tks = slice(c * 64, (c + 1) * 64)

                        def b1(par=par, Hs=Hs, tks=tks):
                            for blk in range(2):
                                P.mm(ps[3][Hs, 0:257], qfm[:, blk, tks], CT[:, blk, :], blk == 0, False, ["qfm", "CT"], ["ps3"], tp=(0, 64 * par))
                            P.mm(ps[3][Hs, 0:257], STs[Hs, :], vu[Hs, :], False, True, [nst, nvu], ["ps3"], signal=True, tp=(64 * par, 64 * par))
                            for blk in range(2):
                                P.mm(ps[4 + blk][:, 0:257], ktm[Hs, blk * 128:(blk + 1) * 128], vu[Hs, :], True, True, [nkt, nvu], [f"ps{4 + blk}"],
                                     signal=True, tp=(64 * par, 0))

                        def b2(par=par, Hs=Hs, c=c):
                            for blk in range(2):
                                P.tt("dve", CT[:, blk, :], CT[:, blk, :], ps[4 + blk][:, 0:257], ALU.add, ["CT", f"ps{4 + blk}"], ["CT"])
                            P.ts("dve", CT[:].rearrange("p b n -> p (b n)"), CT[:].rearrange("p b n -> p (b n)"), gB[:, h, c:c + 1], None,
                                 ALU.mult, None, ["CT", "gB"], ["CT"])
                            P.ts("dve", rd[Hs, :], ps[3][Hs, 256:257], colsT[Hs, jj, 32 + h:33 + h], None, ALU.max, None, ["ps3", "colsT"], ["rd"])
                            P.stt(rd[Hs, :], ps[3][Hs, 256:257], -1.0, rd[Hs, :], ALU.mult, ALU.max, ["ps3", "rd"], ["rd"])
                            P.op("dve", (lambda e, Hs=Hs: e.reciprocal(rd[Hs, :], rd[Hs, :])), ["rd"], ["rd"])
                            P.ts("dve", hh[Hs, :], ps[3][Hs, 0:256], rd[Hs, 0:1], None, ALU.mult, None, ["ps3", "rd"], ["hh"])
                            P.act(junk[Hs, :], hh[Hs, :], AF.Square, ["hh"], ["junk2", "ssq"], accum_out=ssq[Hs, 0:1])
                            P.rsqrt(rn[Hs, :], ssq[Hs, :], 1.0 / 256, HEAD_NORM_EPS, ["ssq"], ["rn"])
                            P.stt(yv[Hs, :], hh[Hs, :], rn[Hs, 0:1], ngb[Hs, h * 256:(h + 1) * 256], ALU.mult, ALU.mult, ["hh", "rn", "ngb"], ["yv"])
                            P.tt("dve", yv[Hs, :], yv[Hs, :], sgo[Hs, :], ALU.mult, ["yv", nsg], ["yv"])
                        hops += [b1, b2]

                    def b3():
                        if yml_d is not None:
                            P.dma("sp", yml_d[tk, h * 256:(h + 1) * 256], yv[:], ["yv"], ())
                        for fb in range(2):
                            P.tr(ps[6][:, fb * 128:(fb + 1) * 128], yv[:, fb * 128:(fb + 1) * 128], identf[:], ["yv", "identf"], ["ps6"], signal=(fb == 1))
                        half = (jj * 128) // NTOK
                        c0 = (jj * 128) % NTOK
                        ydst = yT[:, 8 + 2 * h:10 + 2 * h, c0:c0 + 128]
                        ysrc = ps[6][:, 0:256].rearrange("p (f t) -> p f t", f=2)
                        if half == 0:
                            P.ts("dve", ydst, ysrc, sel[:, 0:1], None, ALU.mult, None, ["ps6", "sel"], ["yT"])
                        else:
                            P.stt(ydst, ysrc, sel[:, 1:2], ydst, ALU.mult, ALU.add, ["ps6", "sel", "yT"], ["yT"])
                    hops.append(b3)
                    return hops

                for jj in range(17):
                    A_ = A_stages(jj) if jj < 16 else []
                    B_ = B_hops(jj - 1) if jj >= 1 else []
                    for k in range(max(len(A_), len(B_))):
                        if k < len(B_):
                            B_[k]()
                        if k < len(A_):
                            A_[k]()
            P.barrier()
    P.barrier()


def ffn_phase(nc, P, SB, ps, hT, yT, identf, identb, din, mod_d, out_d, dbg, dbg_tensor):
    xm = din("xm", [NTOK, D])
    w_out_blk = din("w_out_blk", [4, 128, KC, 512])
    n2g = din("n2g", [1, D])
    fgd = din("fg", [1, D])
    rw_d = din("router_w_r", [128, KC, NE])
    rb_d = din("router_b", [1, NE])
    bgu_d = din("bgu_cols", [128, NE, 32])
    bdn_d = din("b_dn", [NE, D])
    nexp = 1 if "one_expert" in dbg else NE
    wgu_d = din("moe_w_gu", [nexp, 16, 128, KC, 256])
    wdn_d = din("moe_w_dn", [nexp, 8, 128, KC, 256])
    x1_d = nc.dram_tensor("x1_d", [NTOK, D], F32, kind="ExternalOutput" if "x1" in dbg else "Internal").ap()
    NT = NTOK // 128
    h2T = yT
    acc = hT[:].bitcast(F32).rearrange("p a b -> p (a b)").rearrange("p (i d) -> p i d", d=D)
    with contextlib.ExitStack() as st:
        gw = SB(st, "gw", [128, NT, NE])
        with contextlib.ExitStack() as st2:
            gtm = SB(st2, "gtm", [128, D])
            wbs = [SB(st2, f"wo_bf{i}", [128, KC, 512], BF16) for i in range(2)]
            xt = [SB(st2, f"xo{i}", [128, 512]) for i in range(2)]
            tm = [SB(st2, f"xtm{i}", [128, 512]) for i in range(2)]
            P.dma("sp", gtm[:], mod_d[0:1, 2 * D:3 * D].partition_broadcast(128), (), ["gtm"])
            for nb in range(4):
                nsl = slice(nb * 512, (nb + 1) * 512)
                wb, wbn_ = wbs[nb % 2], f"wo_bf{nb % 2}"
                if nb == 0:
                    P.dma("pool", wbs[0][:], w_out_blk[0], (), ["wo_bf0"])
                if nb + 1 < 4:
                    P.dma("pool", wbs[(nb + 1) % 2][:], w_out_blk[nb + 1], (), [f"wo_bf{(nb + 1) % 2}"])
                for i in range(NT):
                    x_, xn = xt[i % 2], f"xo{i % 2}"
                    t_, tn = tm[i % 2], f"xtm{i % 2}"
                    pb, pn = ps[i % 2], f"ps{i % 2}"
                    P.dma("sp", x_[:], xm[i * 128:(i + 1) * 128, nsl], (), [xn])
                    for kc in range(KC):
                        P.mm(pb[:, :], yT[:, kc, i * 128:(i + 1) * 128], wb[:, kc, :], kc == 0, kc == KC - 1, [wbn_], [pn],
                             signal=(kc == KC - 1))
                    P.tt("dve", t_[:], pb[:, :], gtm[:, nsl], ALU.mult, [pn, "gtm"], [tn])
                    P.tt("dve", t_[:], t_[:], x_[:], ALU.add, [tn, xn], [tn])
                    P.dma("sp", x1_d[i * 128:(i + 1) * 128, nsl], t_[:], [tn], ["x1_d"])
            P.barrier()

        if "stop5a" in dbg:
            return
        with contextlib.ExitStack() as st2:
            g2s = SB(st2, "g2s", [128, D]); shf = SB(st2, "shf", [128, D]); n2b = SB(st2, "n2b", [128, D])
            xt = [SB(st2, f"x1t{i}", [128, D]) for i in range(2)]
            junk = SB(st2, "junk3", [128, D])
            ss = SB(st2, "ss2", [128, NT]); rstd = SB(st2, "rstd2", [128, NT])
            rw = SB(st2, "rw", [128, KC, NE]); rbb = SB(st2, "rbb", [128, NE])
            h2s = SB(st2, "h2s", [128, KC, 128])
            lg = SB(st2, "lgts", [128, NE]); m8 = SB(st2, "m8", [128, 8]); msk = SB(st2, "msk", [128, NE])
            nmx = SB(st2, "nmx", [128, 1]); esum = SB(st2, "esum", [128, 1])
            P.dma("sp", g2s[:], mod_d[0:1, 4 * D:5 * D].partition_broadcast(128), (), ["g2s"])
            P.dma("sp", shf[:], mod_d[0:1, 3 * D:4 * D].partition_broadcast(128), (), ["shf"])
            P.dma("sp", n2b[:], n2g[0:1, :].partition_broadcast(128), (), ["n2b"])
            P.dma("sp", rw[:], rw_d, (), ["rw"])
            P.dma("sp", rbb[:], rb_d[0:1, :].partition_broadcast(128), (), ["rbb"])
            P.stt(g2s[:], g2s[:], 1.0, n2b[:], ALU.add, ALU.mult, ["g2s", "n2b"], ["g2s"])
            P.memset("pool", ss[:], 0.0, ["ss2"])
            for i in range(NT):
                x_, xn = xt[i % 2], f"x1t{i % 2}"
                P.dma("sp", x_[:], x1_d[i * 128:(i + 1) * 128, :], ["x1_d"], [xn])
                P.act(junk[:], x_[:], AF.Square, [xn], ["junk3", "ss2"], accum_out=ss[:, i:i + 1])
                P.rsqrt(rstd[:, i:i + 1], ss[:, i:i + 1], 1.0 / D, RMS_EPS, ["ss2"], ["rstd2"])
                P.stt(x_[:], x_[:], rstd[:, i:i + 1], g2s[:], ALU.mult, ALU.mult, [xn, "rstd2", "g2s"], [xn])
                P.tt("dve", x_[:], x_[:], shf[:], ALU.add, [xn, "shf"], [xn])
                for grp in range(4):
                    pb, pn = ps[grp], f"ps{grp}"
                    for q in range(4):
                        kc = grp * 4 + q
                        P.tr(pb[:, q * 128:(q + 1) * 128], x_[:, kc * 128:(kc + 1) * 128], identf[:], [xn, "identf"], [pn], signal=(q == 3))
                    P.cp("act", h2T[:, grp * 4:(grp + 1) * 4, i * 128:(i + 1) * 128], pb[:, :].rearrange("p (k t) -> p k t", k=4), [pn], ["h2T"])
                    P.cp("dve", h2s[:, grp * 4:(grp + 1) * 4, :], pb[:, :].rearrange("p (k t) -> p k t", k=4), [pn, "h2T"], ["h2s"])
                if "no_router" in dbg:
                    continue
                if "no_rmm" not in dbg:
                    for kc in range(KC):
                        P.mm(ps[4][:, 0:NE], h2s[:, kc, :], rw[:, kc, :], kc == 0, kc == KC - 1, ["h2s", "rw"], ["ps4"], signal=(kc == KC - 1))
                if "no_topk" in dbg:
                    continue
                P.tt("dve", lg[:], ps[4][:, 0:NE], rbb[:], ALU.add, ["ps4", "rbb"], ["lgts"])
                P.op("dve", lambda e: e.max(out=m8[:], in_=lg[:]), ["lgts"], ["m8"])
                P.ts("dve", msk[:], lg[:], m8[:, 3:4], None, ALU.is_ge, None, ["lgts", "m8"], ["msk"])
                P.ts("dve", nmx[:], m8[:, 0:1], -1.0, None, ALU.mult, None, ["m8"], ["nmx"])
                P.act(lg[:], lg[:], AF.Exp, ["lgts", "nmx"], ["lgts"], bias=nmx[:, 0:1])
                P.tt("dve", lg[:], lg[:], msk[:], ALU.mult, ["lgts", "msk"], ["lgts"])
                P.op("dve", lambda e: e.tensor_reduce(esum[:], lg[:], AX.X, ALU.add), ["lgts"], ["esum"])
                P.op("dve", lambda e: e.reciprocal(esum[:], esum[:]), ["esum"], ["esum"])
                P.ts("dve", gw[:, i, :], lg[:], esum[:, 0:1], None, ALU.mult, None, ["lgts", "esum"], ["gw"])
            if "gw" in dbg:
                P.dma("sp", dbg_tensor("gw", [128, NT, NE]), gw[:], ["gw"], ())
                P.dma("sp", dbg_tensor("h2T", [128, KC, NTOK], BF16), h2T[:], ["h2T"], ())
            P.barrier()

        if "stop5b" in dbg:
            return
        with contextlib.ExitStack() as st2:
            actT = SB(st2, "actT", [128, KC, NTOK], BF16)
            bgu = SB(st2, "bgu", [128, NE, 32])
            st_i = contextlib.ExitStack()
            bdn = SB(st_i, "bdn", [NE, D])
            gwT = SB(st_i, "gwT", [NE, NT, 128])
            P.dma("sp", bgu[:], bgu_d, (), ["bgu"])
            P.dma("sp", bdn[:], bdn_d, (), ["bdn"])
            for i in range(NT):
                P.tr(ps[i // 4][0:NE, (i % 4) * 128:(i % 4 + 1) * 128], gw[:, i, :], identf[:], ["identf"], [f"ps{i // 4}"], signal=(i % 4 == 3))
            for hf in range(2):
                P.cp("dve", gwT[:, hf * 4:(hf + 1) * 4, :].rearrange("e i t -> e (i t)"), ps[hf][0:NE, 0:512], [f"ps{hf}"], ["gwT"])
            for i in range(NT):
                for nb in range(4):
                    pb, pn = ps[1 + nb % 2], f"ps{1 + nb % 2}"
                    P.mm(pb[:, :], gwT[:, i, :], bdn[:, nb * 512:(nb + 1) * 512], True, True, ["gwT", "bdn"], [pn], signal=True)
                    P.cp("act" if nb % 2 else "dve", acc[:, i, nb * 512:(nb + 1) * 512], pb[:, :], [pn], [f"acc{i}"])
            P.barrier()
            st_i.close()
            NBUF = 6
            wbf = [SB(st2, f"mw_bf{i}", [128, KC, 256], BF16) for i in range(NBUF)]
            gt_ = [SB(st2, f"mg{i}", [128, 512]) for i in range(2)]
            sg_ = [SB(st2, f"msg{i}", [128, 512]) for i in range(2)]
            ut_ = [SB(st2, f"mu{i}", [128, 512]) for i in range(2)]
            blocks = []
            for e in range(nexp):
                for cb in range(8):
                    blocks.append(wgu_d[e, cb])
                    blocks.append(wgu_d[e, 8 + cb])
                for cb in range(8):
                    blocks.append(wdn_d[e, cb])
            issued = [0]

            def prefetch(k):
                lim = min(len(blocks), k + NBUF)
                while issued[0] < lim:
                    j = issued[0]
                    P.dma("pool", wbf[j % NBUF][:], blocks[j], (), [f"mw_bf{j % NBUF}"])
                    issued[0] += 1

            def wb_(k):
                return wbf[k % NBUF], f"mw_bf{k % NBUF}"

            kblk = 0
            for e in range(nexp):
                for cb in range(8):
                    prefetch(kblk)
                    wg, wgn = wb_(kblk)
                    wu, wun = wb_(kblk + 1)
                    kblk += 2
                    for sub in range(2):
                        fb = cb * 2 + sub
                        for th in range(2):
                            tk = slice(th * 512, (th + 1) * 512)
                            k2 = (sub * 2 + th) % 2
                            pg, pgn = ps[2 * k2], f"ps{2 * k2}"
                            pu, pun = ps[2 * k2 + 1], f"ps{2 * k2 + 1}"
                            for kc in range(KC):
                                P.mm(pg[:, :], wg[:, kc, sub * 128:(sub + 1) * 128], h2T[:, kc, tk], kc == 0, kc == KC - 1, [wgn], [pgn],
                                     signal=(kc == KC - 1))
                            for kc in range(KC):
                                P.mm(pu[:, :], wu[:, kc, sub * 128:(sub + 1) * 128], h2T[:, kc, tk], kc == 0, kc == KC - 1, [wun], [pun],
                                     signal=(kc == KC - 1))
                            g_, gn = gt_[k2], f"mg{k2}"
                            s2, s2n = sg_[k2], f"msg{k2}"
                            u_, un = ut_[k2], f"mu{k2}"
                            P.ts("dve", g_[:], pg[:, :], bgu[:, e, fb:fb + 1], 7.0, ALU.add, ALU.min, [pgn, "bgu"], [gn])
                            P.act(s2[:], g_[:], AF.Sigmoid, [gn], [s2n], scale=1.702)
                            P.ts("dve", u_[:], pu[:, :], bgu[:, e, 16 + fb:17 + fb], 7.0, ALU.add, ALU.min, [pun, "bgu"], [un])
                            P.ts("dve", u_[:], u_[:], -7.0, 1.0, ALU.max, ALU.add, [un], [un])
                            P.tt("dve", g_[:], g_[:], s2[:], ALU.mult, [gn, s2n], [gn])
                            P.tt("dve", actT[:, fb, tk], u_[:], g_[:], ALU.mult, [un, gn], [f"actT{fb}"])
                for cb in range(8):
                    prefetch(kblk)
                    wd, wdnm = wb_(kblk)
                    kblk += 1
                    for i in range(NT):
                        pb, pn = ps[4 + i % 4], f"ps{4 + i % 4}"
                        for fb in range(KC):
                            P.mm(pb[:, 0:256], actT[:, fb, i * 128:(i + 1) * 128], wd[:, fb, :], fb == 0, fb == KC - 1,
                                 [wdnm, f"actT{fb}"], [pn], signal=(fb == KC - 1))
                        a_ = acc[:, i, cb * 256:(cb + 1) * 256]
                        P.stt(a_, pb[:, 0:256], gw[:, i, e:e + 1], a_, ALU.mult, ALU.add, [pn, f"acc{i}"], [f"acc{i}"])
            if "acc" in dbg:
                for i in range(NT):
                    P.dma("sp", dbg_tensor(f"acc{i}", [128, D]), acc[:, i, :], [f"acc{i}"], ())
            P.barrier()

        if "stop6" in dbg:
            return
        with contextlib.ExitStack() as st2:
            gtf = SB(st2, "gtf", [128, D]); fgb = SB(st2, "fgb", [128, D])
            xt = [SB(st2, f"x2t{i}", [128, D]) for i in range(2)]
            junk = SB(st2, "junk4", [128, D])
            ss = SB(st2, "ss3", [128, NT]); rstd = SB(st2, "rstd3", [128, NT])
            P.dma("sp", gtf[:], mod_d[0:1, 5 * D:6 * D].partition_broadcast(128), (), ["gtf"])
            P.dma("sp", fgb[:], fgd[0:1, :].partition_broadcast(128), (), ["fgb"])
            P.memset("pool", ss[:], 0.0, ["ss3"])
            for i in range(NT):
                x_, xn = xt[i % 2], f"x2t{i % 2}"
                P.dma("sp", x_[:], x1_d[i * 128:(i + 1) * 128, :], (), [xn])
                P.tt("pool", acc[:, i, :], acc[:, i, :], gtf[:], ALU.mult, ["gtf", f"acc{i}"], [f"acc{i}"])
                P.tt("dve", x_[:], x_[:], acc[:, i, :], ALU.add, [xn, f"acc{i}"], [xn])
                P.act(junk[:], x_[:], AF.Square, [xn], ["junk4", "ss3"], accum_out=ss[:, i:i + 1])
                P.rsqrt(rstd[:, i:i + 1], ss[:, i:i + 1], 1.0 / D, RMS_EPS, ["ss3"], ["rstd3"])
                P.stt(x_[:], x_[:], rstd[:, i:i + 1], fgb[:], ALU.mult, ALU.mult, [xn, "rstd3", "fgb"], [xn])
                P.dma("sp", out_d[i * 128:(i + 1) * 128, :], x_[:], [xn], ["out_d"])
            P.barrier()
    P.barrier()


def build(upto=99, dbg=()):
    nc = bass.Bass("TRN2", target_bir_lowering=False)
    P = Prog(nc)
    dbg_out = {}

    def din(name, shape, dt=F32):
        return nc.dram_tensor(name, list(shape), dt, kind="ExternalInput").ap()

    xb = din("xb", [T, D])
    c128 = din("c128", [128, KC])
    sel_d = din("sel", [128, 2])
    ada_wb = din("ada_wb", [24, 128, KC, 512])
    ada_b = din("ada_b", [1, 6 * D])
    n1g = din("n1g", [1, D])
    w_in_blk = din("w_in_blk", [NWB, 128, KC, 128])
    out_d = nc.dram_tensor("out", [NTOK, D], F32, kind="ExternalOutput").ap()
    mod_d = nc.dram_tensor("mod_d", [1, 6 * D], F32, kind="ExternalOutput" if "mod" in dbg else "Internal").ap()

    def dbg_tensor(name, shape, dt=F32):
        t = nc.dram_tensor("dbg_" + name, list(shape), dt, kind="ExternalOutput").ap()
        dbg_out[name] = t
        return t

    with contextlib.ExitStack() as st0:
        def SB(st, name, shape, dt=F32):
            return st.enter_context(nc.sbuf_tensor("sb_" + name, list(shape), dt))

        ps = [st0.enter_context(nc.psum_tensor(f"ps{i}", [128, 512], F32)) for i in range(8)]
        identb = SB(st0, "identb", [128, 128], BF16)
        identf = SB(st0, "identf", [128, 128], F32)
        ones1 = SB(st0, "ones1", [1, 128], F32)
        sel = SB(st0, "sel", [128, 2], F32)
        hT = SB(st0, "hT", [128, KC, T], BF16)

        P.memset("pool", identf[:], 0.0, ["identf"])
        P.op("pool", lambda e: e.affine_select(identf[:], identf[:], [[-1, 128]], ALU.not_equal, 1.0,
                                               base=0, channel_multiplier=1), ["identf"], ["identf"])
        P.cp("dve", identb[:], identf[:], ["identf"], ["identb"])
        P.memset("pool", ones1[:], 1.0, ["ones1"])
        P.dma("sp", sel[:], sel_d, (), ["sel"])

        with contextlib.ExitStack() as st:
            c_sb = SB(st, "c_sb", [128, KC])
            mrow = [SB(st, f"mrow{i}", [1, 512]) for i in range(2)]
            brow = [SB(st, f"brow{i}", [1, 512]) for i in range(2)]
            scb = SB(st, "scb", [128, KC], BF16)
            wst = [SB(st, f"adaw{i}", [128, KC, 512], BF16) for i in range(3)]
            P.dma("sp", c_sb[:], c128, (), ["c_sb"])
            P.act(scb[:], c_sb[:], AF.Silu, ["c_sb"], ["scb"])
            for nb in range(24):
                w = wst[nb % 3]
                wn = f"adaw{nb % 3}"
                P.dma("pool", w[:], ada_wb[nb], (), [wn])
                P.dma("sp", brow[nb % 2][:], ada_b[0:1, nb * 512:(nb + 1) * 512], (), [f"brow{nb % 2}"])
                pb = ps[nb % 2]
                pn = f"ps{nb % 2}"
                for kc in range(KC):
                    P.mm(pb[0:1, :], scb[:, kc:kc + 1], w[:, kc, :], kc == 0, kc == KC - 1, ["scb", wn], [pn], signal=(kc == KC - 1))
                P.tt("dve", mrow[nb % 2][0:1, :], pb[0:1, :], brow[nb % 2][0:1, :], ALU.add, [pn, f"brow{nb % 2}"], [f"mrow{nb % 2}"])
                P.dma("sp", mod_d[0:1, nb * 512:(nb + 1) * 512], mrow[nb % 2][:], [f"mrow{nb % 2}"], ["mod_d"])
            P.barrier()

        with contextlib.ExitStack() as st:
            g1s = SB(st, "g1s", [128, D])
            shm = SB(st, "shm", [128, D])
            n1b = SB(st, "n1b", [128, D])
            xt = [SB(st, f"xt{i}", [128, D]) for i in range(2)]
            hb = [SB(st, f"hb{i}", [128, D], BF16) for i in range(2)]
            junk = SB(st, "junk", [128, D])
            ss = SB(st, "ss", [128, 16])
            rstd = SB(st, "rstd", [128, 16])
            P.memset("pool", ss[:], 0.0, ["ss"])
            P.dma("sp", g1s[:], mod_d[0:1, D:2 * D].partition_broadcast(128), ["mod_d"], ["g1s"])
            P.dma("sp", shm[:], mod_d[0:1, 0:D].partition_broadcast(128), ["mod_d"], ["shm"])
            P.dma("sp", n1b[:], n1g[0:1, :].partition_broadcast(128), (), ["n1b"])
            P.stt(g1s[:], g1s[:], 1.0, n1b[:], ALU.add, ALU.mult, ["g1s", "n1b"], ["g1s"])
            for i in range(16):
                x_ = xt[i % 2]
                xn = f"xt{i % 2}"
                h_ = hb[i % 2]
                hn = f"hb{i % 2}"
                P.dma("sp", x_[:], xb[i * 128:(i + 1) * 128, :], (), [xn])
                P.act(junk[:], x_[:], AF.Square, [xn], ["junk", "ss"], accum_out=ss[:, i:i + 1])
                P.rsqrt(rstd[:, i:i + 1], ss[:, i:i + 1], 1.0 / D, RMS_EPS, ["ss"], ["rstd"])
                P.stt(x_[:], x_[:], rstd[:, i:i + 1], g1s[:], ALU.mult, ALU.mult, [xn, "rstd", "g1s"], [xn])
                P.tt("dve", h_[:], x_[:], shm[:], ALU.add, [xn, "shm"], [hn])
                for half in range(2):
                    pb = ps[(2 * i + half) % 4]
                    pn = f"ps{(2 * i + half) % 4}"
                    pbb = pb[:].bitcast(BF16)
                    for q in range(8):
                        kc = half * 8 + q
                        P.tr(pbb[:, q * 128:(q + 1) * 128], h_[:, kc * 128:(kc + 1) * 128], identb[:],
                             [hn, "identb"], [pn], signal=(q == 7))
                    P.cp("act" if half else "dve", hT[:, half * 8:(half + 1) * 8, i * 128:(i + 1) * 128],
                         pbb[:, 0:1024].rearrange("p (k t) -> p k t", k=8), [pn], [f"hT{i}"])
            if "hT" in dbg:
                P.dma("sp", dbg_tensor("hT", [128, KC, T], BF16), hT[:], [f"hT{i}" for i in range(16)], ())
            P.barrier()

        yT = SB(st0, "yT", [128, KC, NTOK], BF16)
        blockones = SB(st0, "blockones", [128, 128])
        P.memset("pool", blockones[:], 0.0, ["blockones"])
        P.memset("pool", blockones[0:64, 0:64], 1.0, ["blockones"])
        P.memset("pool", blockones[64:128, 64:128], 1.0, ["blockones"])

        if upto >= 3 and "skip_rwkv" not in dbg:
            rwkv_phase(nc, P, SB, ps, hT, yT, identf, identb, blockones, sel, w_in_blk, din, dbg, dbg_tensor)
        if upto >= 4 and "skip_mlstm" not in dbg:
            mlstm_phase(nc, P, SB, ps, hT, yT, identf, identb, sel, w_in_blk, din, dbg, dbg_tensor)
        if "yT" in dbg:
            P.dma("sp", dbg_tensor("yT", [128, KC, NTOK], BF16), yT[:], ["yT"], ())
            P.barrier()
        if upto >= 5:
            ffn_phase(nc, P, SB, ps, hT, yT, identf, identb, din, mod_d, out_d, dbg, dbg_tensor)

        P.barrier()
        P.emit()
    return nc, dbg_out


def prep_inputs(inputs):
    f = lambda a: np.ascontiguousarray(a, dtype=np.float32)
    shared = {}
    ada_w = inputs["ada_w"][0]
    shared["ada_wb"] = f(ada_w.reshape(KC, 128, 24, 512).transpose(2, 1, 0, 3))
    shared["ada_b"] = f(inputs["ada_b"])
    shared["n1g"] = f(inputs["norm1_g"])
    w_in = inputs["w_in"][0]
    idx = w_in_col_index()
    wpad = np.concatenate([w_in, np.zeros((D, 1), np.float32)], axis=1)
    blk = wpad[:, np.where(idx < 0, w_in.shape[1], idx)]
    shared["w_in_blk"] = f(blk.reshape(KC, 128, NWB, 128).transpose(2, 1, 0, 3))
    mu = inputs["rwkv_mu"][0]
    chs = lambda p: np.concatenate([64 * p + np.arange(64), 64 * (p + 8) + np.arange(64)])
    vecs = [mu[0:1024], mu[1088:2112], mu[2112:3136], inputs["rwkv_w0"][0], inputs["rwkv_a0"][0], inputs["rwkv_kk"][0],
            inputs["rwkv_ka"][0], inputs["rwkv_rk"][0].reshape(1024), inputs["rwkv_ln_w"][0], inputs["rwkv_ln_b"][0]]
    shared["rw_cols"] = f(np.stack([np.stack([v[chs(p)] for v in vecs], -1) for p in range(8)], 1))
    lcols = np.zeros((128, 3), np.float32)
    lcols[:64, 0] = mu[1024:1088]
    lcols[64:, 0] = mu[3136:3200]
    lcols[:, 1] = mu[3200:3328]
    lcols[:32, 2] = mu[3328:3360]
    shared["lora_cols"] = lcols
    w2, a2, g2 = inputs["rwkv_w2"][0], inputs["rwkv_a2"][0], inputs["rwkv_g2"][0]
    shared["wa2"] = f(np.stack([np.concatenate([w2[:, chs(p)], a2[:, chs(p)]], 0) for p in range(8)], 1))
    shared["g2a"] = f(np.stack([g2[0:128][:, chs(p)] for p in range(8)], 1))
    shared["g2b"] = f(np.stack([g2[128:160][:, chs(p)] for p in range(8)], 1))
    cw, cb = inputs["mlstm_conv_w"][0], inputs["mlstm_conv_b"][0]
    mlc = np.zeros((128, 16, 5), np.float32)
    for i in range(16):
        mlc[:, i, 0:4] = cw[:, i * 128:(i + 1) * 128].T
        mlc[:, i, 4] = cb[i * 128:(i + 1) * 128]
    shared["ml_cols"] = mlc
    shared["ml_gb"] = f(np.stack([inputs["mlstm_b_i"][0], inputs["mlstm_b_f"][0]], 1))
    shared["ml_ng"] = f(inputs["mlstm_norm_g"])
    chs_all = np.concatenate([chs(p) for p in range(8)] + [1024 + np.arange(1024)])
    shared["w_out_blk"] = f(inputs["w_out"][0][chs_all].reshape(KC, 128, 4, 512).transpose(2, 1, 0, 3))
    shared["n2g"] = f(inputs["norm2_g"])
    shared["fg"] = f(inputs["final_g"].reshape(1, D))
    shared["router_w_r"] = f(inputs["router_w"][0].reshape(KC, 128, NE).transpose(1, 0, 2))
    shared["router_b"] = f(inputs["router_b"])
    shared["bgu_cols"] = f(inputs["moe_b_gu"][0].reshape(NE, 32, 128).transpose(2, 0, 1))
    shared["b_dn"] = f(inputs["moe_b_dn"][0])
    if "moe_w_gu" in inputs:
        g_ = inputs["moe_w_gu"][0]
        ne = g_.shape[0]
        shared["moe_w_gu"] = f(g_.reshape(ne, KC, 128, 16, 256).transpose(0, 3, 2, 1, 4))
        d_ = inputs["moe_w_dn"][0]
        shared["moe_w_dn"] = f(d_.reshape(ne, KC, 128, 8, 256).transpose(0, 3, 2, 1, 4))
    per_core = []
    for core in range(8):
        b, j = core // 2, core % 2
        d = dict(shared)
        d["xb"] = f(inputs["x"][b])
        d["xm"] = f(inputs["x"][b, j * NTOK:(j + 1) * NTOK])
        d["c128"] = f(inputs["c"][b].reshape(KC, 128).T)
        s = np.zeros((128, 2), np.float32)
        s[:, j] = 1.0
        d["sel"] = s
        per_core.append(d)
    return per_core


def kernel(**inputs):
    nc, _ = build()
    in_maps = prep_inputs(inputs)
    res = run_bass_kernel_spmd(nc, in_maps, core_ids=list(range(8)))
    out = np.zeros((4, T, D), np.float32)
    for core in range(8):
        b, j = core // 2, core % 2
        out[b, j * NTOK:(j + 1) * NTOK] = res.results[core]["out"]
    return out
```

```python
import contextlib
import numpy as np
import concourse.bass as bass
import concourse.mybir as mybir
from concourse.bass_utils import run_bass_kernel_spmd

F32 = mybir.dt.float32
BF16 = mybir.dt.bfloat16
ALU = mybir.AluOpType
AF = mybir.ActivationFunctionType
AX = mybir.AxisListType

SEMCH = 30000
NDMA = 8
NO_SELF_WAIT = False

T = 2048
D = 2048
KC = 16
RMS_EPS = 1e-5
GN_EPS = 64e-5
HEAD_NORM_EPS = 1e-6
NE = 32
FF = 2048
NTOK = 1024


class Prog:
    ENGS = ("pe", "dve", "act", "pool", "sp")

    def __init__(self, nc):
        self.nc = nc
        self.ops = {e: [] for e in self.ENGS}
        self.nsig = {e: 0 for e in self.ENGS}
        self.ndma = {e: 0 for e in self.ENGS}
        self.known = {e: {} for e in self.ENGS}
        self.lastw = {}
        self.readers = {}
        self.pending = {e: False for e in self.ENGS}

    def _need(self, eng, stamp, waits):
        kind, f, s = stamp
        if kind == 'e':
            if f == eng and (eng == "pe" or NO_SELF_WAIT):
                return
            key = ('e', f)
            if self.known[eng].get(key, 0) >= s:
                return
            assert s <= self.nsig[f], f"wait on pending stamp {stamp} from {eng}"
            self.known[eng][key] = s
            waits.append(stamp)
        else:
            slot = s % NDMA
            cnt = s // NDMA + 1
            key = ('d', f, slot)
            if self.known[eng].get(key, 0) >= cnt:
                return
            self.known[eng][key] = cnt
            waits.append(stamp)

    def _deps(self, eng, reads, writes):
        waits = []
        for r in reads:
            w = self.lastw.get(r)
            if w is not None:
                self._need(eng, w, waits)
        for r in writes:
            w = self.lastw.get(r)
            if w is not None:
                self._need(eng, w, waits)
            for st in self.readers.get(r, ()):
                self._need(eng, st, waits)
        return waits

    def _record(self, stamp, reads, writes):
        for r in reads:
            self.readers.setdefault(r, []).append(stamp)
        for r in writes:
            self.lastw[r] = stamp
            self.readers[r] = []

    def op(self, eng, fn, reads=(), writes=(), signal=True):
        waits = self._deps(eng, reads, writes)
        seq = self.nsig[eng] + 1
        if signal:
            self.nsig[eng] = seq
            self.pending[eng] = False
        else:
            self.pending[eng] = True
        self.ops[eng].append((waits, fn, 'op', signal))
        self._record(('e', eng, seq), reads, writes)

    def dma(self, eng, out, in_, reads=(), writes=()):
        waits = self._deps(eng, reads, writes)
        idx = self.ndma[eng]
        self.ndma[eng] = idx + 1
        if idx >= NDMA:
            self._need(eng, ('d', eng, idx - NDMA), waits)
        self.ops[eng].append((waits, (out, in_), 'dma', idx))
        self._record(('d', eng, idx), reads, writes)

    def dma_custom(self, eng, fn, reads=(), writes=()):
        waits = self._deps(eng, reads, writes)
        idx = self.ndma[eng]
        self.ndma[eng] = idx + 1
        if idx >= NDMA:
            self._need(eng, ('d', eng, idx - NDMA), waits)
        self.ops[eng].append((waits, fn, 'dmac', idx))
        self._record(('d', eng, idx), reads, writes)

    def flush(self, eng):
        if self.pending[eng]:
            self.op(eng, None, signal=True)

    def barrier(self):
        for e in self.ENGS:
            self.flush(e)
        snap = dict(self.nsig)
        dsnap = dict(self.ndma)
        for e in self.ENGS:
            waits = []
            for f in self.ENGS:
                if f != e and snap[f] > 0:
                    self._need(e, ('e', f, snap[f]), waits)
                n = dsnap[f]
                for i in range(max(0, n - NDMA), n):
                    self._need(e, ('d', f, i), waits)
            if waits:
                self.ops[e].append((waits, None, 'nop', False))
        self.lastw = {}
        self.readers = {}

    def emit(self):
        nc = self.nc
        with contextlib.ExitStack() as st:
            esem = {}
            for e in self.ENGS:
                n = (self.nsig[e] + SEMCH - 1) // SEMCH
                esem[e] = [st.enter_context(nc.semaphore(f"s_{e}{i}")) for i in range(max(n, 1))]
            dsem = {}
            for e in self.ENGS:
                if self.ndma[e]:
                    dsem[e] = [st.enter_context(nc.semaphore(f"d_{e}{i}")) for i in range(NDMA)]
            block = st.enter_context(nc.Block())

            def run(engname, engh):
                seq = 0
                for waits, fn, kind, extra in self.ops[engname]:
                    for (k, f, s) in waits:
                        if k == 'e':
                            engh.wait_ge(esem[f][(s - 1) // SEMCH], (s - 1) % SEMCH + 1)
                        else:
                            engh.wait_ge(dsem[f][s % NDMA], 16 * (s // NDMA + 1))
                    if kind == 'op':
                        ins = engh.nop() if fn is None else fn(engh)
                        if extra:
                            seq += 1
                            ins.then_inc(esem[engname][(seq - 1) // SEMCH], 1)
                    elif kind == 'dma':
                        out, in_ = fn
                        engh.dma_start(out=out, in_=in_).then_inc(dsem[engname][extra % NDMA], 16)
                    elif kind == 'dmac':
                        fn(engh).then_inc(dsem[engname][extra % NDMA], 16)

            @block.tensor
            def _(e):
                run("pe", e)

            @block.vector
            def _(e):
                run("dve", e)

            @block.scalar
            def _(e):
                run("act", e)

            @block.gpsimd
            def _(e):
                run("pool", e)

            @block.sync
            def _(e):
                run("sp", e)

    def mm(self, out, lhsT, rhs, start, stop, reads, writes, signal=False, tp=None):
        self.op("pe", lambda e: e.matmul(out, lhsT, rhs, start=start, stop=stop, tile_position=tp),
                reads, writes, signal)

    def tr(self, out, in_, ident, reads, writes, signal=False, tp=None):
        self.op("pe", lambda e: e.transpose(out, in_, ident, tile_position=tp), reads, writes, signal)

    def cp(self, eng, out, in_, reads, writes):
        if eng == "act":
            self.op(eng, lambda e: e.copy(out, in_), reads, writes)
        else:
            self.op(eng, lambda e: e.tensor_copy(out, in_), reads, writes)

    def tt(self, eng, out, a, b, op, reads, writes):
        self.op(eng, lambda e: e.tensor_tensor(out, a, b, op), reads, writes)

    def ts(self, eng, out, a, s1, s2, op0, op1, reads, writes):
        if s2 is None:
            self.op(eng, lambda e: e.tensor_scalar(out, a, s1, None, op0), reads, writes)
        else:
            self.op(eng, lambda e: e.tensor_scalar(out, a, s1, s2, op0, op1), reads, writes)

    def stt(self, out, a, s, b, op0, op1, reads, writes):
        self.op("dve", lambda e: e.scalar_tensor_tensor(out, a, s, b, op0, op1), reads, writes)

    def act(self, out, in_, func, reads, writes, bias=0.0, scale=1.0, accum_out=None):
        if accum_out is None:
            self.op("act", lambda e: e.activation(out, in_, func, bias=bias, scale=scale), reads, writes)
        else:
            self.op("act", lambda e: e.activation(out, in_, func, bias=bias, scale=scale, accum_out=accum_out),
                    reads, writes)

    def rsqrt(self, out, in_, scale, bias, reads, writes):
        self.op("act", lambda e: e.activation(out, in_, AF.Sqrt, bias=bias, scale=scale), reads, writes)
        self.op("dve", lambda e: e.reciprocal(out, out), writes, writes)

    def memset(self, eng, ap, val, writes):
        self.op(eng, lambda e: e.memset(ap, val), (), writes)


NWB = 60


def w_in_col_index():
    idx = np.zeros((NWB, 128), np.int64) - 1
    for p in range(8):
        for j, base in enumerate((0, 1088, 2112)):
            cols = np.concatenate([base + 64 * p + np.arange(64), base + 64 * (p + 8) + np.arange(64)])
            idx[3 * p + j] = cols
    idx[24, :64] = 1024 + np.arange(64)
    idx[24, 64:] = 3136 + np.arange(64)
    idx[25] = 3200 + np.arange(128)
    idx[26, :32] = 3328 + np.arange(32)
    for g, base in enumerate((3360, 4384, 5408, 6432)):
        for i in range(8):
            idx[27 + 8 * g + i] = base + 128 * i + np.arange(128)
    idx[59, :8] = 7456 + np.arange(8)
    return idx


NCH = 4
SEG = NCH * 64
NSEG = T // SEG


def rwkv_phase(nc, P, SB, ps, hT, yT, identf, identb, blockones, sel, w_in_blk, din, dbg, dbg_tensor):
    rw_cols_d = din("rw_cols", [128, 8, 10])
    lora_cols_d = din("lora_cols", [128, 3])
    wa2_d = din("wa2", [128, 8, 128])
    g2a_d = din("g2a", [128, 8, 128])
    g2b_d = din("g2b", [32, 8, 128])
    W = SEG
    yrw_d = dbg_tensor("yrw", [8, 128, T]) if "yrw" in dbg else None
    with contextlib.ExitStack() as st:
        lo_a = SB(st, "lo_a", [128, T])
        sg0 = SB(st, "sg0", [128, T])
        sg1 = SB(st, "sg1", [32, T], BF16)
        wa2 = SB(st, "wa2", [128, 8, 128])
        g2a = SB(st, "g2a", [128, 8, 128])
        g2b = SB(st, "g2b", [32, 8, 128], BF16)
        rwc = SB(st, "rwc", [128, 8, 10])
        rwd = SB(st, "rwd", [128, 8, 6])
        lc = SB(st, "lc", [128, 3])
        lcd = SB(st, "lcd", [128, 3])
        P.dma("sp", wa2[:], wa2_d, (), ["wa2"])
        P.dma("sp", g2a[:], g2a_d, (), ["g2a"])
        P.dma("pool", g2b[:], g2b_d, (), ["g2b"])
        P.dma("sp", rwc[:], rw_cols_d, (), ["rwc"])
        P.dma("sp", lc[:], lora_cols_d, (), ["lc"])
        for j in range(3):
            P.ts("dve", rwd[:, :, j], rwc[:, :, j], -1.0, 1.0, ALU.mult, ALU.add, ["rwc"], ["rwd"])
        P.ts("dve", rwd[:, :, 3], rwc[:, :, 3], -1.0, None, ALU.mult, None, ["rwc"], ["rwd"])
        P.ts("dve", rwd[:, :, 4], rwc[:, :, 4], -1.0, None, ALU.mult, None, ["rwc"], ["rwd"])
        P.ts("dve", rwd[:, :, 5], rwc[:, :, 6], -1.0, 1.0, ALU.mult, ALU.add, ["rwc"], ["rwd"])
        P.ts("dve", lcd[:], lc[:], -1.0, 1.0, ALU.mult, ALU.add, ["lc"], ["lcd"])

        with contextlib.ExitStack() as st2:
            wbf = SB(st2, "wbf", [128, KC, 128], BF16)
            raw = SB(st2, "raw", [128, T])
            tmp = SB(st2, "tmp", [128, T])
            for bi, (blk, dest, npart, dn) in enumerate(((24, lo_a, 128, "lo_a"), (25, sg0, 128, "sg0"), (26, sg1, 32, "sg1"))):
                P.dma("pool", wbf[:], w_in_blk[blk], (), ["wbf"])
                for tb in range(4):
                    pb, pn = ps[tb % 2], f"ps{tb % 2}"
                    for kc in range(KC):
                        P.mm(pb[:, :], wbf[:, kc, :], hT[:, kc, tb * 512:(tb + 1) * 512], kc == 0, kc == KC - 1,
                             ["wbf"], [pn], signal=(kc == KC - 1))
                    P.cp("dve" if tb % 2 else "act", raw[:, tb * 512:(tb + 1) * 512], pb[:, :], [pn], ["raw"])
                n = npart
                P.ts("dve", tmp[0:n, :], raw[0:n, :], lcd[0:n, bi:bi + 1], None, ALU.mult, None, ["raw", "lcd"], ["tmp"])
                P.stt(tmp[0:n, 1:T], raw[0:n, 0:T - 1], lc[0:n, bi:bi + 1], tmp[0:n, 1:T], ALU.mult, ALU.add,
                      ["raw", "lc", "tmp"], ["tmp"])
                if blk == 24:
                    P.act(dest[0:64, :], tmp[0:64, :], AF.Tanh, ["tmp"], [dn])
                    P.cp("act", dest[64:128, :], tmp[64:128, :], ["tmp"], [dn])
                else:
                    P.act(dest[0:n, :], tmp[0:n, :], AF.Sigmoid, ["tmp"], [dn])
            if "lora" in dbg:
                P.dma("sp", dbg_tensor("lo_a", [128, T]), lo_a[:], ["lo_a"], ())
                P.dma("sp", dbg_tensor("sg0", [128, T]), sg0[:], ["sg0"], ())
            P.barrier()

        with contextlib.ExitStack() as st3:
            def TL(name, w=W, dt=F32):
                return SB(st3, name, [128, w], dt)
            wps = [SB(st3, f"wp{i}", [128, KC, 3, 128], BF16) for i in range(2)]
            mSU = TL("mSU"); mU = TL("mU"); mSL = TL("mSL"); idr = TL("idr"); ones_t = TL("ones_t", 64)
            praw = [TL(f"praw{j}", W + 1) for j in range(3)]
            sh = [TL(f"sh{j}") for j in range(3)]
            tq = [TL(f"tq{j}") for j in range(8)]
            Bt = TL("Bt", dt=BF16); Kt = TL("Kt", dt=BF16); Bh = TL("Bh", dt=BF16); Kh = TL("Kh", dt=BF16)
            Qm = TL("Qm", dt=BF16); QTm = TL("QTm", dt=BF16); AkT = TL("AkT", dt=BF16); Vb = TL("Vb", dt=BF16)
            DB = {}
            for nm in ("At", "Rt", "Xm", "ArbT", "ArkT", "Vtm", "Bhtm", "Khtm"):
                DB[nm] = [TL(f"{nm}{i}", dt=BF16) for i in range(2)]
            for nm in ("AkV", "bonus", "gfm"):
                DB[nm] = [TL(f"{nm}{i}") for i in range(2)]
            DB["gamL"] = [SB(st3, f"gamL{i}", [128, NCH]) for i in range(2)]
            gy = TL("gy"); gq = TL("gq")
            ST = SB(st3, "ST", [128, 64], BF16)
            Zt = SB(st3, "Zt", [128, 64], BF16)
            Ut = SB(st3, "Ut", [128, 64], BF16)

            for (m, mn, pat, cm, cmp_) in ((mSU, "mSU", [[0, NCH], [1, 64]], -1, ALU.is_gt),
                                          (mU, "mU", [[0, NCH], [1, 64]], -1, ALU.is_ge),
                                          (mSL, "mSL", [[0, NCH], [-1, 64]], 1, ALU.is_gt),
                                          (idr, "idr", [[0, NCH], [1, 64]], -1, ALU.is_equal)):
                P.memset("pool", m[:], 1.0, [mn])
                for hp in range(2):
                    sl = slice(hp * 64, (hp + 1) * 64)
                    P.op("pool", (lambda e, m=m, sl=sl, pat=pat, cm=cm, cmp_=cmp_:
                                  e.affine_select(m[sl, :], m[sl, :], pat, cmp_, 0.0, base=0, channel_multiplier=cm)),
                         [mn], [mn])
            P.memset("pool", ones_t[:], 1.0, ["ones_t"])
            rst = TL("rst")
            P.memset("pool", rst[:], 1.0, ["rst"])
            for c in range(NCH):
                P.memset("pool", rst[:, c * 64:c * 64 + 1], 0.0, ["rst"])

            H = [slice(0, 64), slice(64, 128)]
            TP = [(0, 0), (64, 64)]

            def C(c):
                return slice(c * 64, (c + 1) * 64)

            def cmm(bank, lhs, ln, rhs, rn):
                for c in range(NCH):
                    for hp in range(2):
                        P.mm(ps[bank][H[hp], C(c)], lhs[H[hp], C(c)], rhs[H[hp], C(c)], True, True, [ln, rn], [f"ps{bank}"],
                             signal=(c == NCH - 1 and hp == 1), tp=TP[hp])

            def pre_stages(u):
                p, s_ = divmod(u, NSEG)
                par = u % 2
                t0 = s_ * W
                col = lambda j: rwc[:, p, j:j + 1]
                dcol = lambda j: rwd[:, p, j:j + 1]
                wp, wpn = wps[p % 2], f"wp{p % 2}"
                At, Rt, Xm, AkV, ArbT, ArkT = (DB[n][par] for n in ("At", "Rt", "Xm", "AkV", "ArbT", "ArkT"))
                Vtm, Bhtm, Khtm, bonus, gfm, gamL = (DB[n][par] for n in ("Vtm", "Bhtm", "Khtm", "bonus", "gfm", "gamL"))
                nAt, nRt, nXm, nAkV, nArbT, nArkT = (f"{n}{par}" for n in ("At", "Rt", "Xm", "AkV", "ArbT", "ArkT"))
                nVtm, nBhtm, nKhtm, nbonus, ngfm, ngamL = (f"{n}{par}" for n in ("Vtm", "Bhtm", "Khtm", "bonus", "gfm", "gamL"))
                shr, shk, shv = sh
                st_ = []

                def S(f):
                    st_.append(f)
                    return f

                @S
                def _():
                    if s_ == 0:
                        for pp in ([0, 1] if p == 0 else ([p + 1] if p + 1 < 8 else [])):
                            for j in range(3):
                                P.dma("pool", wps[pp % 2][:, :, j, :], w_in_blk[3 * pp + j], (), [f"wp{pp % 2}"])
                        for j in range(3):
                            P.memset("dve", praw[j][:, 0:1], 0.0, [f"praw{j}"])
                    for j in range(3):
                        for kc in range(KC):
                            P.mm(ps[j][:, 0:W], wp[:, kc, j, :], hT[:, kc, t0:t0 + W], kc == 0, kc == KC - 1,
                                 [wpn], [f"ps{j}"], signal=(kc == KC - 1))

                @S
                def _():
                    for j in range(3):
                        P.cp("act", praw[j][:, 1:W + 1], ps[j][:, 0:W], [f"ps{j}"], [f"praw{j}"])
                        P.ts("dve", tq[0][:], praw[j][:, 1:W + 1], dcol(j), None, ALU.mult, None, [f"praw{j}", "rwd"], ["tq0"])
                        P.stt(sh[j][:], praw[j][:, 0:W], col(j), tq[0][:], ALU.mult, ALU.add, [f"praw{j}", "rwc", "tq0"], [f"sh{j}"])
                        P.cp("dve", praw[j][:, 0:1], praw[j][:, W:W + 1], [f"praw{j}"], [f"praw{j}"])
                    P.mm(ps[3][:, 0:W], wa2[0:64, p, :], lo_a[0:64, t0:t0 + W], True, True, ["wa2"], ["ps3"], signal=True)
                    P.mm(ps[0][:, 0:W], wa2[64:128, p, :], lo_a[64:128, t0:t0 + W], True, True, ["wa2"], ["ps0"], signal=True)
                    P.mm(ps[1][:, 0:W], g2a[:, p, :], sg0[:, t0:t0 + W], True, False, ["g2a"], ["ps1"])
                    P.mm(ps[1][:, 0:W], g2b[0:32, p, :], sg1[0:32, t0:t0 + W], False, True, ["g2b"], ["ps1"], signal=True)

                @S
                def _():
                    P.act(tq[1][:], ps[3][:, 0:W], AF.Exp, ["ps3", "rwd"], ["tq1"], bias=dcol(3), scale=-1.0)
                    P.act(tq[1][:], tq[1][:], AF.Ln, ["tq1"], ["tq1"], bias=1.0)
                    P.act(tq[1][:], tq[1][:], AF.Exp, ["tq1"], ["tq1"], bias=-0.5, scale=-1.0)
                    P.ts("dve", tq[3][:], shk[:], col(5), None, ALU.mult, None, ["sh1", "rwc"], ["tq3"])
                    P.tt("dve", tq[4][:], tq[3][:], tq[3][:], ALU.mult, ["tq3"], ["tq4"])
                    P.mm(ps[2][:, 0:W], blockones[:], tq[4][:], True, True, ["blockones", "tq4"], ["ps2"], signal=True)
                    P.act(tq[2][:], ps[0][:, 0:W], AF.Exp, ["ps0", "rwd"], ["tq2"], bias=dcol(4), scale=-1.0)
                    P.act(tq[2][:], tq[2][:], AF.Ln, ["tq2"], ["tq2"], bias=1.0)
                    P.act(tq[2][:], tq[2][:], AF.Exp, ["tq2"], ["tq2"], scale=-1.0)
                    P.cp("act", gfm[:], ps[1][:, 0:W], ["ps1"], [ngfm])

                @S
                def _():
                    P.ts("dve", tq[4][:], ps[2][:, 0:W], 1e-24, None, ALU.max, None, ["ps2"], ["tq4"])
                    P.act(tq[4][:], tq[4][:], AF.Sqrt, ["tq4"], ["tq4"])
                    P.op("dve", lambda e: e.reciprocal(tq[4][:], tq[4][:]), ["tq4"], ["tq4"])
                    P.tt("dve", tq[3][:], tq[3][:], tq[4][:], ALU.mult, ["tq3", "tq4"], ["tq3"])
                    P.ts("dve", tq[4][:], tq[2][:], col(6), dcol(5), ALU.mult, ALU.add, ["tq2", "rwc", "rwd"], ["tq4"])
                    P.tt("dve", tq[4][:], shk[:], tq[4][:], ALU.mult, ["sh1", "tq4"], ["tq4"])
                    P.tt("dve", tq[2][:], tq[3][:], tq[2][:], ALU.mult, ["tq3", "tq2"], ["tq2"])
                    P.stt(tq[5][:], shr[:], col(7), tq[4][:], ALU.mult, ALU.mult, ["sh0", "rwc", "tq4"], ["tq5"])
                    P.mm(ps[3][:, 0:W], blockones[:], tq[5][:], True, True, ["blockones", "tq5"], ["ps3"], signal=True)
                    P.tt("dve", bonus[:], ps[3][:, 0:W], shv[:], ALU.mult, ["ps3", "sh2"], [nbonus])

                @S
                def _():
                    P.op("dve", lambda e: e.tensor_tensor_scan(tq[5][:], rst[:], tq[1][:], 0.0, ALU.mult, ALU.subtract),
                         ["rst", "tq1"], ["tq5"])
                    lg3 = tq[5][:].rearrange("p (c t) -> p c t", t=64)
                    P.act(tq[6][:], tq[5][:], AF.Exp, ["tq5"], ["tq6"])
                    P.tt("dve", tq[7][:], tq[5][:], tq[1][:], ALU.add, ["tq5", "tq1"], ["tq7"])
                    P.tt("dve", Rt[:], shr[:], tq[6][:], ALU.mult, ["sh0", "tq6"], [nRt])
                    P.act(tq[7][:], tq[7][:], AF.Exp, ["tq7"], ["tq7"])
                    P.act(tq[6][:], tq[5][:], AF.Exp, ["tq5"], ["tq6"], scale=-1.0)
                    P.stt(At[:], tq[3][:], -1.0, tq[7][:], ALU.mult, ALU.mult, ["tq3", "tq7"], [nAt])
                    P.tt("dve", tq[7][:].rearrange("p (c t) -> p c t", t=64), lg3[:, :, 63:64].to_broadcast([128, NCH, 64]),
                         lg3, ALU.subtract, ["tq5"], ["tq7"])
                    P.act(gamL[:], lg3[:, :, 63], AF.Exp, ["tq5"], [ngamL])
                    P.act(tq[7][:], tq[7][:], AF.Exp, ["tq7"], ["tq7"])
                    P.tt("dve", Bt[:], tq[2][:], tq[6][:], ALU.mult, ["tq2", "tq6"], ["Bt"])
                    P.tt("dve", Kt[:], tq[4][:], tq[6][:], ALU.mult, ["tq4", "tq6"], ["Kt"])
                    P.tt("dve", Bh[:], tq[2][:], tq[7][:], ALU.mult, ["tq2", "tq7"], ["Bh"])
                    P.tt("dve", Kh[:], tq[4][:], tq[7][:], ALU.mult, ["tq4", "tq7"], ["Kh"])

                @S
                def _():
                    P.cp("act", Vb[:], shv[:], ["sh2"], ["Vb"])
                    for (src, sn, bank) in ((Bh, "Bh", 0), (Kh, "Kh", 1), (Vb, "Vb", 2)):
                        for c in range(NCH):
                            for hp in range(2):
                                P.mm(ps[bank][H[hp], C(c)], src[H[hp], C(c)], identb[H[hp], H[hp]], True, True, [sn], [f"ps{bank}"],
                                     signal=(c == NCH - 1 and hp == 1), tp=TP[hp])
                    cmm(3, Bt, "Bt", At, nAt)

                @S
                def _():
                    P.cp("act", Bhtm[:], ps[0][:, 0:W], ["ps0"], [nBhtm])
                    P.cp("act", Khtm[:], ps[1][:, 0:W], ["ps1"], [nKhtm])
                    P.cp("act", Vtm[:], ps[2][:, 0:W], ["ps2"], [nVtm])
                    P.tt("dve", Qm[:], ps[3][:, 0:W], mSU[:], ALU.mult, ["ps3", "mSU"], ["Qm"])
                    P.tt("dve", Xm[:], Qm[:], idr[:], ALU.add, ["Qm", "idr"], [nXm])
                    cmm(0, At, nAt, Bt, "Bt")
                    cmm(1, Kt, "Kt", At, nAt)
                    cmm(2, Bt, "Bt", Rt, nRt)
                    cmm(3, Kt, "Kt", Rt, nRt)

                @S
                def _():
                    P.tt("dve", QTm[:], ps[0][:, 0:W], mSL[:], ALU.mult, ["ps0", "mSL"], ["QTm"])
                    P.tt("dve", AkT[:], ps[1][:, 0:W], mSU[:], ALU.mult, ["ps1", "mSU"], ["AkT"])
                    P.tt("dve", ArbT[:], ps[2][:, 0:W], mU[:], ALU.mult, ["ps2", "mU"], [nArbT])
                    P.tt("dve", ArkT[:], ps[3][:, 0:W], mU[:], ALU.mult, ["ps3", "mU"], [nArkT])
                    cmm(0, AkT, "AkT", Vtm, nVtm)
                    cmm(1, QTm, "QTm", Qm, "Qm")
                    cmm(2, Qm, "Qm", QTm, "QTm")

                @S
                def _():
                    P.cp("act", AkV[:], ps[0][:, 0:W], ["ps0"], [nAkV])
                    P.cp("act", Qm[:], ps[1][:, 0:W], ["ps1"], ["Qm"])
                    P.cp("act", QTm[:], ps[2][:, 0:W], ["ps2"], ["QTm"])

                for it in range(5):
                    def mmstep(it=it):
                        cmm(3, QTm, "QTm", Xm, nXm)
                        if it < 4:
                            cmm(1, QTm, "QTm", Qm, "Qm")
                            cmm(2, Qm, "Qm", QTm, "QTm")
                    def evstep(it=it):
                        P.tt("dve", Xm[:], Xm[:], ps[3][:, 0:W], ALU.add, [nXm, "ps3"], [nXm])
                        if it < 4:
                            P.cp("act", Qm[:], ps[1][:, 0:W], ["ps1"], ["Qm"])
                            P.cp("act", QTm[:], ps[2][:, 0:W], ["ps2"], ["QTm"])
                    st_.append(mmstep)
                    st_.append(evstep)
                return st_

            def chain_hops(u):
                p, s_ = divmod(u, NSEG)
                par = u % 2
                t0 = s_ * W
                col = lambda j: rwc[:, p, j:j + 1]
                At, Rt, Xm, AkV, ArbT, ArkT = (DB[n][par] for n in ("At", "Rt", "Xm", "AkV", "ArbT", "ArkT"))
                Vtm, Bhtm, Khtm, bonus, gfm, gamL = (DB[n][par] for n in ("Vtm", "Bhtm", "Khtm", "bonus", "gfm", "gamL"))
                nAt, nRt, nXm, nAkV, nArbT, nArkT = (f"{n}{par}" for n in ("At", "Rt", "Xm", "AkV", "ArbT", "ArkT"))
                nVtm, nBhtm, nKhtm, nbonus, ngfm, ngamL = (f"{n}{par}" for n in ("Vtm", "Bhtm", "Khtm", "bonus", "gfm", "gamL"))
                hops = []
                for c in range(NCH):
                    def h1(c=c):
                        if s_ == 0 and c == 0:
                            P.memset("dve", ST[:], 0.0, ["ST"])
                        for hp in range(2):
                            P.mm(ps[4][H[hp], C(c)], At[H[hp], C(c)], ST[H[hp], :], True, True, [nAt, "ST"], ["ps4"],
                                 signal=(hp == 1), tp=TP[hp])
                    def h2(c=c):
                        P.tt("dve", Zt[:], ps[4][:, C(c)], AkV[:, C(c)], ALU.add, ["ps4", nAkV], ["Zt"])
                        for hp in range(2):
                            P.mm(ps[5][H[hp], C(c)], Xm[H[hp], C(c)], Zt[H[hp], :], True, True, [nXm, "Zt"], ["ps5"],
                                 signal=(hp == 1), tp=TP[hp])
                    def h3(c=c):
                        P.cp("act", Ut[:], ps[5][:, C(c)], ["ps5"], ["Ut"])
                        for hp in range(2):
                            P.mm(ps[7][H[hp], C(c)], ST[H[hp], :], Rt[H[hp], C(c)], True, False, ["ST", nRt], ["ps7"], tp=TP[hp])
                            P.mm(ps[7][H[hp], C(c)], Ut[H[hp], :], ArbT[H[hp], C(c)], False, False, ["Ut", nArbT], ["ps7"], tp=TP[hp])
                            P.mm(ps[7][H[hp], C(c)], Vtm[H[hp], C(c)], ArkT[H[hp], C(c)], False, True, [nVtm, nArkT], ["ps7"], tp=TP[hp])
                        for hp in range(2):
                            P.mm(ps[6][H[hp], C(c)], Bhtm[H[hp], C(c)], Ut[H[hp], :], True, False, [nBhtm, "Ut"], ["ps6"], tp=TP[hp])
                            P.mm(ps[6][H[hp], C(c)], Khtm[H[hp], C(c)], Vtm[H[hp], C(c)], False, True, [nKhtm, nVtm], ["ps6"],
                                 signal=(hp == 1), tp=TP[hp])
                    def h4(c=c):
                        P.stt(ST[:], ST[:], gamL[:, c:c + 1], ps[6][:, C(c)], ALU.mult, ALU.add, ["ST", ngamL, "ps6"], ["ST"])
                    hops += [h1, h2, h3, h4]

                def g1():
                    P.cp("act", gy[:], ps[7][:, 0:W], ["ps7"], ["gy"])
                    P.mm(ps[4][:, 0:W], blockones[:], gy[:], True, True, ["blockones", "gy"], ["ps4"], signal=True)
                def g2():
                    P.stt(gy[:], ps[4][:, 0:W], -1.0 / 64, gy[:], ALU.mult, ALU.add, ["ps4", "gy"], ["gy"])
                    P.tt("dve", gq[:], gy[:], gy[:], ALU.mult, ["gy"], ["gq"])
                    P.mm(ps[5][:, 0:W], blockones[:], gq[:], True, True, ["blockones", "gq"], ["ps5"], signal=True)
                def g3():
                    P.act(gq[:], ps[5][:, 0:W], AF.Ln, ["ps5"], ["gq"], bias=GN_EPS, scale=1.0 / 64)
                    P.act(gq[:], gq[:], AF.Exp, ["gq"], ["gq"], scale=-0.5)
                    P.tt("dve", gy[:], gy[:], gq[:], ALU.mult, ["gy", "gq"], ["gy"])
                    P.ts("dve", gy[:], gy[:], col(8), col(9), ALU.mult, ALU.add, ["gy", "rwc"], ["gy"])
                    P.tt("dve", gy[:], gy[:], bonus[:], ALU.add, ["gy", nbonus], ["gy"])
                    P.tt("dve", gy[:], gy[:], gfm[:], ALU.mult, ["gy", ngfm], ["gy"])
                    if yrw_d is not None:
                        P.dma("sp", yrw_d[p][:, t0:t0 + W], gy[:], ["gy"], ())
                    half = t0 // NTOK
                    c0 = t0 % NTOK
                    if half == 0:
                        P.ts("dve", yT[:, p, c0:c0 + W], gy[:], sel[:, 0:1], None, ALU.mult, None, ["gy", "sel"], ["yT"])
                    else:
                        P.stt(yT[:, p, c0:c0 + W], gy[:], sel[:, 1:2], yT[:, p, c0:c0 + W], ALU.mult, ALU.add,
                              ["gy", "sel", "yT"], ["yT"])
                hops += [g1, g2, g3]
                return hops

            NU = 8 * NSEG
            for u in range(NU + 1):
                A_ = pre_stages(u) if u < NU else []
                B_ = chain_hops(u - 1) if u >= 1 else []
                for k in range(max(len(A_), len(B_))):
                    if k < len(A_):
                        A_[k]()
                    if k < len(B_):
                        B_[k]()
            P.barrier()
    P.barrier()


def mlstm_phase(nc, P, SB, ps, hT, yT, identf, identb, sel, w_in_blk, din, dbg, dbg_tensor):
    ml_cols_d = din("ml_cols", [128, 16, 5])
    ml_gb_d = din("ml_gb", [4, 2])
    ml_ng_d = din("ml_ng", [1, 1024])
    yml_d = dbg_tensor("yml", [T, 1024]) if "yml" in dbg else None
    H = [slice(0, 64), slice(64, 128)]
    with contextlib.ExitStack() as st:
        mlc = SB(st, "mlc", [128, 16, 5])
        gb = SB(st, "gb", [4, 2])
        ngb = SB(st, "ngb", [128, 1024])
        colsT = SB(st, "colsT", [128, 16, 36])
        gB = SB(st, "gB", [128, 4, 32])
        mUm = SB(st, "mUm", [128, 64])
        wbfs = [SB(st, f"wbf2_{i}", [128, KC, 128], BF16) for i in range(2)]
        wbf = wbfs[0]
        P.dma("sp", mlc[:], ml_cols_d, (), ["mlc"])
        P.dma("sp", gb[:], ml_gb_d, (), ["gb"])
        P.dma("sp", ngb[:], ml_ng_d[0:1, :].partition_broadcast(128), (), ["ngb"])
        P.memset("pool", mUm[:], 1.0, ["mUm"])
        for hp in range(2):
            P.op("pool", (lambda e, hp=hp: e.affine_select(mUm[H[hp], :], mUm[H[hp], :], [[1, 64]], ALU.is_ge, 0.0,
                                                           base=0, channel_multiplier=-1)), ["mUm"], ["mUm"])
        with contextlib.ExitStack() as st2:
            A = SB(st2, "gA", [4, T]); B = SB(st2, "gB_", [4, T]); Fb = SB(st2, "gF", [4, T])
            Rrow = SB(st2, "Rrow", [128, T])
            onesr = SB(st2, "onesr", [4, T])
            Me = SB(st2, "Me", [4, 32]); gr = SB(st2, "gr", [4, 32]); nbf = SB(st2, "nbf", [4, 1])
            selrows = SB(st2, "selrows", [4, 4, 128])
            P.memset("pool", Rrow[:], 0.0, ["Rrow"])
            P.memset("pool", onesr[:], 1.0, ["onesr"])
            P.memset("pool", Me[:], 0.0, ["Me"])
            P.ts("dve", nbf[:], gb[:, 1:2], -1.0, None, ALU.mult, None, ["gb"], ["nbf"])
            for h in range(4):
                P.cp("dve", selrows[0:4, h, :], identf[0:4, h:h + 1].to_broadcast([4, 128]), ["identf"], ["selrows"])
            P.dma("pool", wbf[:], w_in_blk[59], (), ["wbf2"])
            for tb in range(4):
                for kc in range(KC):
                    P.mm(ps[0][0:4, :], wbf[:, kc, 0:4], hT[:, kc, tb * 512:(tb + 1) * 512], kc == 0, kc == KC - 1, ["wbf2"], ["ps0"])
                for kc in range(KC):
                    P.mm(ps[1][0:4, :], wbf[:, kc, 4:8], hT[:, kc, tb * 512:(tb + 1) * 512], kc == 0, kc == KC - 1, ["wbf2"], ["ps1"],
                         signal=(kc == KC - 1))
                sl = slice(tb * 512, (tb + 1) * 512)
                P.ts("dve", A[:, sl], ps[0][0:4, :], gb[:, 0:1], None, ALU.add, None, ["ps0", "gb"], ["gA"])
                P.act(B[:, sl], ps[1][0:4, :], AF.Exp, ["ps1", "nbf"], ["gB_"], bias=nbf[:, 0:1], scale=-1.0)
            P.act(B[:], B[:], AF.Ln, ["gB_"], ["gB_"], bias=1.0)
            P.ts("dve", B[:], B[:], -1.0, None, ALU.mult, None, ["gB_"], ["gB_"])
            P.op("dve", lambda e: e.tensor_tensor_scan(Fb[:], onesr[:], B[:], 0.0, ALU.mult, ALU.add), ["onesr", "gB_"], ["gF"])
            P.tt("dve", A[:], A[:], Fb[:], ALU.subtract, ["gA", "gF"], ["gA"])
            P.op("dve", lambda e: e.tensor_tensor_scan(B[:], onesr[:], A[:], 0.0, ALU.mult, ALU.max), ["onesr", "gA"], ["gB_"])
            M3 = B[:].rearrange("p (c t) -> p c t", t=64)
            A3 = A[:].rearrange("p (c t) -> p c t", t=64)
            F3 = Fb[:].rearrange("p (c t) -> p c t", t=64)
            P.cp("dve", Me[:, 1:32], M3[:, 0:31, 63], ["gB_"], ["Me"])
            P.tt("dve", gr[:], Me[:], M3[:, :, 63], ALU.subtract, ["Me", "gB_"], ["gr"])
            P.act(gr[:], gr[:], AF.Exp, ["gr"], ["gr"])
            Meb = Me[:].unsqueeze(2).to_broadcast([4, 32, 64])
            R0 = Rrow[0:4, :].rearrange("p (c t) -> p c t", t=64)
            R1 = Rrow[32:36, :].rearrange("p (c t) -> p c t", t=64)
            P.tt("dve", R0, A3, Meb, ALU.subtract, ["gA", "Me"], ["Rrow"])
            P.act(Rrow[0:4, :], Rrow[0:4, :], AF.Exp, ["Rrow"], ["Rrow"])
            P.tt("dve", R1, F3, Meb, ALU.add, ["gF", "Me"], ["Rrow"])
            P.act(Rrow[32:36, :], Rrow[32:36, :], AF.Exp, ["Rrow"], ["Rrow"], scale=-1.0)
            for jj in range(16):
                pb, pn = ps[2 + jj % 2], f"ps{2 + jj % 2}"
                P.tr(pb[:, 0:128], Rrow[:, jj * 128:(jj + 1) * 128], identf[:], ["Rrow", "identf"], [pn], signal=True)
                P.cp("act" if jj % 2 else "dve", colsT[:, jj, :], pb[:, 0:36], [pn], ["colsT"])
            for h in range(4):
                P.mm(ps[4][:, h * 32:(h + 1) * 32], selrows[0:4, h, :], gr[0:4, :], True, True, ["selrows", "gr"], ["ps4"], signal=(h == 3))
            P.cp("dve", gB[:].rearrange("p h c -> p (h c)"), ps[4][:, 0:128], ["ps4"], ["gB"])
            P.barrier()

        with contextlib.ExitStack() as st3:
            qfm = SB(st3, "qfm", [128, 2, T], BF16); kfm = SB(st3, "kfm", [128, 2, T], BF16)
            cacc = SB(st3, "cacc", [128, T])
            rawc = SB(st3, "rawc", [128, T + 3])
            wv = SB(st3, "wv", [128, KC, 256], BF16); wo = SB(st3, "wo", [128, KC, 256], BF16)
            CT = SB(st3, "CT", [128, 2, 257], BF16)
            ktms = [SB(st3, f"ktm{i}", [128, 256], BF16) for i in range(2)]
            vaugs = [SB(st3, f"vaug{i}", [128, 257]) for i in range(2)]
            sgos = [SB(st3, f"sgo{i}", [128, 256]) for i in range(2)]
            STss = [SB(st3, f"STs{i}", [128, 64], BF16) for i in range(2)]
            vus = [SB(st3, f"vu{i}", [128, 257], BF16) for i in range(2)]
            hh = SB(st3, "hh", [128, 256]); yv = SB(st3, "yv", [128, 256]); junk = SB(st3, "junk2", [128, 256])
            rd = SB(st3, "rd", [128, 1]); ssq = SB(st3, "ssq", [128, 1]); rn = SB(st3, "rn", [128, 1])
            P.memset("pool", rawc[:, 0:3], 0.0, ["rawc"])
            for i in range(2):
                P.memset("pool", vaugs[i][:, 256:257], 1.0, [f"vaug{i}"])
            for h in range(4):
                for which, dst, dn in ((0, qfm, "qfm"), (1, kfm, "kfm")):
                    for blk in range(2):
                        bi = 27 + 8 * which + 2 * h + blk
                        ci = 8 * which + 2 * h + blk
                        wbf, wbn = wbfs[ci % 2], f"wbf2_{ci % 2}"
                        P.dma("pool", wbf[:], w_in_blk[bi], (), [wbn])
                        for tb in range(4):
                            pb, pn = ps[tb % 2], f"ps{tb % 2}"
                            for kc in range(KC):
                                P.mm(pb[:, :], wbf[:, kc, :], hT[:, kc, tb * 512:(tb + 1) * 512], kc == 0, kc == KC - 1, [wbn], [pn],
                                     signal=(kc == KC - 1))
                            P.cp("dve" if tb % 2 else "act", rawc[:, 3 + tb * 512:3 + (tb + 1) * 512], pb[:, :], [pn], ["rawc"])
                        d_ = dst[:, blk, :]
                        P.ts("dve", cacc[:], rawc[:, 3:3 + T], mlc[:, ci, 3:4], mlc[:, ci, 4:5], ALU.mult, ALU.add, ["rawc", "mlc"], ["cacc"])
                        for j in range(3):
                            P.stt(cacc[:], rawc[:, j:j + T], mlc[:, ci, j:j + 1], cacc[:], ALU.mult, ALU.add, ["rawc", "mlc", "cacc"], ["cacc"])
                        P.act(d_, cacc[:], AF.Silu, ["cacc"], [dn])
                        if which == 1:
                            P.ts("dve", d_, d_, 0.0625, None, ALU.mult, None, [dn], [dn])
                for which, dst, dn in ((2, wv, "wv"), (3, wo, "wo")):
                    for blk in range(2):
                        P.dma("pool", dst[:, :, blk * 128:(blk + 1) * 128], w_in_blk[27 + 8 * which + 2 * h + blk], (), [dn])
                P.memset("pool", CT[:], 0.0, ["CT"])

                def A_stages(jj, h=h):
                    q2 = jj % 2
                    vaug, sgo, ktm, STs, vu = vaugs[q2], sgos[q2], ktms[q2], STss[q2], vus[q2]
                    nv, nsg, nkt, nst, nvu = f"vaug{q2}", f"sgo{q2}", f"ktm{q2}", f"STs{q2}", f"vu{q2}"
                    tk = slice(jj * 128, (jj + 1) * 128)
                    st_ = []

                    def s1():
                        for kc in range(KC):
                            P.mm(ps[0][:, 0:256], hT[:, kc, tk], wv[:, kc, :], kc == 0, kc == KC - 1, ["wv"], ["ps0"], signal=(kc == KC - 1))
                        for kc in range(KC):
                            P.mm(ps[1][:, 0:256], hT[:, kc, tk], wo[:, kc, :], kc == 0, kc == KC - 1, ["wo"], ["ps1"], signal=(kc == KC - 1))
                        ps7b = ps[7][:].bitcast(BF16)
                        for blk in range(2):
                            P.tr(ps7b[:, blk * 128:(blk + 1) * 128], kfm[:, blk, tk], identb[:], ["kfm", "identb"], ["ps7"], signal=(blk == 1))
                        for par in range(2):
                            c = 2 * jj + par
                            tks = slice(c * 64, (c + 1) * 64)
                            for blk in range(2):
                                P.mm(ps[2][H[par], 0:64], kfm[:, blk, tks], qfm[:, blk, tks], blk == 0, blk == 1, ["kfm", "qfm"], ["ps2"],
                                     signal=(blk == 1), tp=(0, 64 * par))

                    def s2():
                        P.cp("dve", vaug[:, 0:256], ps[0][:, 0:256], ["ps0"], [nv])
                        P.act(sgo[:], ps[1][:, 0:256], AF.Sigmoid, ["ps1"], [nsg])
                        P.cp("act", ktm[:], ps[7][:].bitcast(BF16)[:, 0:256], ["ps7"], [nkt])
                        P.tt("dve", STs[:], ps[2][:, 0:64], mUm[:], ALU.mult, ["ps2", "mUm"], [nst])
                        for par in range(2):
                            Hs = H[par]
                            P.ts("dve", vu[Hs, :], vaug[Hs, :], colsT[Hs, jj, h:h + 1], None, ALU.mult, None, [nv, "colsT"], [nvu])
                    return [s1, s2]

                def B_hops(jj, h=h):
                    q2 = jj % 2
                    vaug, sgo, ktm, STs, vu = vaugs[q2], sgos[q2], ktms[q2], STss[q2], vus[q2]
                    nv, nsg, nkt, nst, nvu = f"vaug{q2}", f"sgo{q2}", f"ktm{q2}", f"STs{q2}", f"vu{q2}"
                    tk = slice(jj * 128, (jj + 1) * 128)
                    hops = []
                    for par in range(2):
                        c = 2 * jj + par
                        Hs = H[par]
                        tks = slice(c * 64, (c + 1) * 64)

                        def b1(par=par, Hs=Hs, tks=tks):
                            for blk in range(2):
                                P.mm(ps[3][Hs, 0:257], qfm[:, blk, tks], CT[:, blk, :], blk == 0, False, ["qfm", "CT"], ["ps3"], tp=(0, 64 * par))
                            P.mm(ps[3][Hs, 0:257], STs[Hs, :], vu[Hs, :], False, True, [nst, nvu], ["ps3"], signal=True, tp=(64 * par, 64 * par))
                            for blk in range(2):
                                P.mm(ps[4 + blk][:, 0:257], ktm[Hs, blk * 128:(blk + 1) * 128], vu[Hs, :], True, True, [nkt, nvu], [f"ps{4 + blk}"],
                                     signal=True, tp=(64 * par, 0))

                        def b2(par=par, Hs=Hs, c=c):
                            for blk in range(2):
                                P.tt("dve", CT[:, blk, :], CT[:, blk, :], ps[4 + blk][:, 0:257], ALU.add, ["CT", f"ps{4 + blk}"], ["CT"])
                            P.ts("dve", CT[:].rearrange("p b n -> p (b n)"), CT[:].rearrange("p b n -> p (b n)"), gB[:, h, c:c + 1], None,
                                 ALU.mult, None, ["CT", "gB"], ["CT"])
                            P.ts("dve", rd[Hs, :], ps[3][Hs, 256:257], colsT[Hs, jj, 32 + h:33 + h], None, ALU.max, None, ["ps3", "colsT"], ["rd"])
                            P.stt(rd[Hs, :], ps[3][Hs, 256:257], -1.0, rd[Hs, :], ALU.mult, ALU.max, ["ps3", "rd"], ["rd"])
                            P.op("dve", (lambda e, Hs=Hs: e.reciprocal(rd[Hs, :], rd[Hs, :])), ["rd"], ["rd"])
                            P.ts("dve", hh[Hs, :], ps[3][Hs, 0:256], rd[Hs, 0:1], None, ALU.mult, None, ["ps3", "rd"], ["hh"])
                            P.act(junk[Hs, :], hh[Hs, :], AF.Square, ["hh"], ["junk2", "ssq"], accum_out=ssq[Hs, 0:1])
                            P.rsqrt(rn[Hs, :], ssq[Hs, :], 1.0 / 256, HEAD_NORM_EPS, ["ssq"], ["rn"])
                            P.stt(yv[Hs, :], hh[Hs, :], rn[Hs, 0:1], ngb[Hs, h * 256:(h + 1) * 256], ALU.mult, ALU.mult, ["hh", "rn", "ngb"], ["yv"])
                            P.tt("dve", yv[Hs, :], yv[Hs, :], sgo[Hs, :], ALU.mult, ["yv", nsg], ["yv"])
                        hops += [b1, b2]

                    def b3():
                        if yml_d is not None:
                            P.dma("sp", yml_d[tk, h * 256:(h + 1) * 256], yv[:], ["yv"], ())
                        for fb in range(2):
                            P.tr(ps[6][:, fb * 128:(fb + 1) * 128], yv[:, fb * 128:(fb + 1) * 128], identf[:], ["yv", "identf"], ["ps6"], signal=(fb == 1))
                        half = (jj * 128) // NTOK
                        c0 = (jj * 128) % NTOK
                        ydst = yT[:, 8 + 2 * h:10 + 2 * h, c0:c0 + 128]
                        ysrc = ps[6][:, 0:256].rearrange("p (f t) -> p f t", f=2)
                        if half == 0:
                            P.ts("dve", ydst, ysrc, sel[:, 0:1], None, ALU.mult, None, ["ps6", "sel"], ["yT"])
                        else:
                            P.stt(ydst, ysrc, sel[:, 1:2], ydst, ALU.mult, ALU.add, ["ps6", "sel", "yT"], ["yT"])
                    hops.append(b3)
                    return hops

                for jj in range(17):
                    A_ = A_stages(jj) if jj < 16 else []
                    B_ = B_hops(jj - 1) if jj >= 1 else []
                    for k in range(max(len(A_), len(B_))):
                        if k < len(B_):
                            B_[k]()
                        if k < len(A_):
                            A_[k]()
            P.barrier()
    P.barrier()


def ffn_phase(nc, P, SB, ps, hT, yT, identf, identb, din, mod_d, out_d, dbg, dbg_tensor):
    xm = din("xm", [NTOK, D])
    w_out_blk = din("w_out_blk", [4, 128, KC, 512])
    n2g = din("n2g", [1, D])
    fgd = din("fg", [1, D])
    rw_d = din("router_w_r", [128, KC, NE])
    rb_d = din("router_b", [1, NE])
    bgu_d = din("bgu_cols", [128, NE, 32])
    bdn_d = din("b_dn", [NE, D])
    nexp = 1 if "one_expert" in dbg else NE
    wgu_d = din("moe_w_gu", [nexp, 16, 128, KC, 256])
    wdn_d = din("moe_w_dn", [nexp, 8, 128, KC, 256])
    x1_d = nc.dram_tensor("x1_d", [NTOK, D], F32, kind="ExternalOutput" if "x1" in dbg else "Internal").ap()
    NT = NTOK // 128
    h2T = yT
    acc = hT[:].bitcast(F32).rearrange("p a b -> p (a b)").rearrange("p (i d) -> p i d", d=D)
    with contextlib.ExitStack() as st:
        gw = SB(st, "gw", [128, NT, NE])
        with contextlib.ExitStack() as st2:
            gtm = SB(st2, "gtm", [128, D])
            wbs = [SB(st2, f"wo_bf{i}", [128, KC, 512], BF16) for i in range(2)]
            xt = [SB(st2, f"xo{i}", [128, 512]) for i in range(2)]
            tm = [SB(st2, f"xtm{i}", [128, 512]) for i in range(2)]
            P.dma("sp", gtm[:], mod_d[0:1, 2 * D:3 * D].partition_broadcast(128), (), ["gtm"])
            for nb in range(4):
                nsl = slice(nb * 512, (nb + 1) * 512)
                wb, wbn_ = wbs[nb % 2], f"wo_bf{nb % 2}"
                if nb == 0:
                    P.dma("pool", wbs[0][:], w_out_blk[0], (), ["wo_bf0"])
                if nb + 1 < 4:
                    P.dma("pool", wbs[(nb + 1) % 2][:], w_out_blk[nb + 1], (), [f"wo_bf{(nb + 1) % 2}"])
                for i in range(NT):
                    x_, xn = xt[i % 2], f"xo{i % 2}"
                    t_, tn = tm[i % 2], f"xtm{i % 2}"
                    pb, pn = ps[i % 2], f"ps{i % 2}"
                    P.dma("sp", x_[:], xm[i * 128:(i + 1) * 128, nsl], (), [xn])
                    for kc in range(KC):
                        P.mm(pb[:, :], yT[:, kc, i * 128:(i + 1) * 128], wb[:, kc, :], kc == 0, kc == KC - 1, [wbn_], [pn],
                             signal=(kc == KC - 1))
                    P.tt("dve", t_[:], pb[:, :], gtm[:, nsl], ALU.mult, [pn, "gtm"], [tn])
                    P.tt("dve", t_[:], t_[:], x_[:], ALU.add, [tn, xn], [tn])
                    P.dma("sp", x1_d[i * 128:(i + 1) * 128, nsl], t_[:], [tn], ["x1_d"])
            P.barrier()

        if "stop5a" in dbg:
            return
        with contextlib.ExitStack() as st2:
            g2s = SB(st2, "g2s", [128, D]); shf = SB(st2, "shf", [128, D]); n2b = SB(st2, "n2b", [128, D])
            xt = [SB(st2, f"x1t{i}", [128, D]) for i in range(2)]
            junk = SB(st2, "junk3", [128, D])
            ss = SB(st2, "ss2", [128, NT]); rstd = SB(st2, "rstd2", [128, NT])
            rw = SB(st2, "rw", [128, KC, NE]); rbb = SB(st2, "rbb", [128, NE])
            h2s = SB(st2, "h2s", [128, KC, 128])
            lg = SB(st2, "lgts", [128, NE]); m8 = SB(st2, "m8", [128, 8]); msk = SB(st2, "msk", [128, NE])
            nmx = SB(st2, "nmx", [128, 1]); esum = SB(st2, "esum", [128, 1])
            P.dma("sp", g2s[:], mod_d[0:1, 4 * D:5 * D].partition_broadcast(128), (), ["g2s"])
            P.dma("sp", shf[:], mod_d[0:1, 3 * D:4 * D].partition_broadcast(128), (), ["shf"])
            P.dma("sp", n2b[:], n2g[0:1, :].partition_broadcast(128), (), ["n2b"])
            P.dma("sp", rw[:], rw_d, (), ["rw"])
            P.dma("sp", rbb[:], rb_d[0:1, :].partition_broadcast(128), (), ["rbb"])
            P.stt(g2s[:], g2s[:], 1.0, n2b[:], ALU.add, ALU.mult, ["g2s", "n2b"], ["g2s"])
            P.memset("pool", ss[:], 0.0, ["ss2"])
            for i in range(NT):
                x_, xn = xt[i % 2], f"x1t{i % 2}"
                P.dma("sp", x_[:], x1_d[i * 128:(i + 1) * 128, :], ["x1_d"], [xn])
                P.act(junk[:], x_[:], AF.Square, [xn], ["junk3", "ss2"], accum_out=ss[:, i:i + 1])
                P.rsqrt(rstd[:, i:i + 1], ss[:, i:i + 1], 1.0 / D, RMS_EPS, ["ss2"], ["rstd2"])
                P.stt(x_[:], x_[:], rstd[:, i:i + 1], g2s[:], ALU.mult, ALU.mult, [xn, "rstd2", "g2s"], [xn])
                P.tt("dve", x_[:], x_[:], shf[:], ALU.add, [xn, "shf"], [xn])
                for grp in range(4):
                    pb, pn = ps[grp], f"ps{grp}"
                    for q in range(4):
                        kc = grp * 4 + q
                        P.tr(pb[:, q * 128:(q + 1) * 128], x_[:, kc * 128:(kc + 1) * 128], identf[:], [xn, "identf"], [pn], signal=(q == 3))
                    P.cp("act", h2T[:, grp * 4:(grp + 1) * 4, i * 128:(i + 1) * 128], pb[:, :].rearrange("p (k t) -> p k t", k=4), [pn], ["h2T"])
                    P.cp("dve", h2s[:, grp * 4:(grp + 1) * 4, :], pb[:, :].rearrange("p (k t) -> p k t", k=4), [pn, "h2T"], ["h2s"])
                if "no_router" in dbg:
                    continue
                if "no_rmm" not in dbg:
                    for kc in range(KC):
                        P.mm(ps[4][:, 0:NE], h2s[:, kc, :], rw[:, kc, :], kc == 0, kc == KC - 1, ["h2s", "rw"], ["ps4"], signal=(kc == KC - 1))
                if "no_topk" in dbg:
                    continue
                P.tt("dve", lg[:], ps[4][:, 0:NE], rbb[:], ALU.add, ["ps4", "rbb"], ["lgts"])
                P.op("dve", lambda e: e.max(out=m8[:], in_=lg[:]), ["lgts"], ["m8"])
                P.ts("dve", msk[:], lg[:], m8[:, 3:4], None, ALU.is_ge, None, ["lgts", "m8"], ["msk"])
                P.ts("dve", nmx[:], m8[:, 0:1], -1.0, None, ALU.mult, None, ["m8"], ["nmx"])
                P.act(lg[:], lg[:], AF.Exp, ["lgts", "nmx"], ["lgts"], bias=nmx[:, 0:1])
                P.tt("dve", lg[:], lg[:], msk[:], ALU.mult, ["lgts", "msk"], ["lgts"])
                P.op("dve", lambda e: e.tensor_reduce(esum[:], lg[:], AX.X, ALU.add), ["lgts"], ["esum"])
                P.op("dve", lambda e: e.reciprocal(esum[:], esum[:]), ["esum"], ["esum"])
                P.ts("dve", gw[:, i, :], lg[:], esum[:, 0:1], None, ALU.mult, None, ["lgts", "esum"], ["gw"])
            if "gw" in dbg:
                P.dma("sp", dbg_tensor("gw", [128, NT, NE]), gw[:], ["gw"], ())
                P.dma("sp", dbg_tensor("h2T", [128, KC, NTOK], BF16), h2T[:], ["h2T"], ())
            P.barrier()

        if "stop5b" in dbg:
            return
        with contextlib.ExitStack() as st2:
            actT = SB(st2, "actT", [128, KC, NTOK], BF16)
            bgu = SB(st2, "bgu", [128, NE, 32])
            st_i = contextlib.ExitStack()
            bdn = SB(st_i, "bdn", [NE, D])
            gwT = SB(st_i, "gwT", [NE, NT, 128])
            P.dma("sp", bgu[:], bgu_d, (), ["bgu"])
            P.dma("sp", bdn[:], bdn_d, (), ["bdn"])
            for i in range(NT):
                P.tr(ps[i // 4][0:NE, (i % 4) * 128:(i % 4 + 1) * 128], gw[:, i, :], identf[:], ["identf"], [f"ps{i // 4}"], signal=(i % 4 == 3))
            for hf in range(2):
                P.cp("dve", gwT[:, hf * 4:(hf + 1) * 4, :].rearrange("e i t -> e (i t)"), ps[hf][0:NE, 0:512], [f"ps{hf}"], ["gwT"])
            for i in range(NT):
                for nb in range(4):
                    pb, pn = ps[1 + nb % 2], f"ps{1 + nb % 2}"
                    P.mm(pb[:, :], gwT[:, i, :], bdn[:, nb * 512:(nb + 1) * 512], True, True, ["gwT", "bdn"], [pn], signal=True)
                    P.cp("act" if nb % 2 else "dve", acc[:, i, nb * 512:(nb + 1) * 512], pb[:, :], [pn], [f"acc{i}"])
            P.barrier()
            st_i.close()
            NBUF = 6
            wbf = [SB(st2, f"mw_bf{i}", [128, KC, 256], BF16) for i in range(NBUF)]
            gt_ = [SB(st2, f"mg{i}", [128, 512]) for i in range(2)]
            sg_ = [SB(st2, f"msg{i}", [128, 512]) for i in range(2)]
            ut_ = [SB(st2, f"mu{i}", [128, 512]) for i in range(2)]
            blocks = []
            for e in range(nexp):
                for cb in range(8):
                    blocks.append(wgu_d[e, cb])
                    blocks.append(wgu_d[e, 8 + cb])
                for cb in range(8):
                    blocks.append(wdn_d[e, cb])
            issued = [0]

            def prefetch(k):
                lim = min(len(blocks), k + NBUF)
                while issued[0] < lim:
                    j = issued[0]
                    P.dma("pool", wbf[j % NBUF][:], blocks[j], (), [f"mw_bf{j % NBUF}"])
                    issued[0] += 1

            def wb_(k):
                return wbf[k % NBUF], f"mw_bf{k % NBUF}"

            kblk = 0
            for e in range(nexp):
                for cb in range(8):
                    prefetch(kblk)
                    wg, wgn = wb_(kblk)
                    wu, wun = wb_(kblk + 1)
                    kblk += 2
                    for sub in range(2):
                        fb = cb * 2 + sub
                        for th in range(2):
                            tk = slice(th * 512, (th + 1) * 512)
                            k2 = (sub * 2 + th) % 2
                            pg, pgn = ps[2 * k2], f"ps{2 * k2}"
                            pu, pun = ps[2 * k2 + 1], f"ps{2 * k2 + 1}"
                            for kc in range(KC):
                                P.mm(pg[:, :], wg[:, kc, sub * 128:(sub + 1) * 128], h2T[:, kc, tk], kc == 0, kc == KC - 1, [wgn], [pgn],
                                     signal=(kc == KC - 1))
                            for kc in range(KC):
                                P.mm(pu[:, :], wu[:, kc, sub * 128:(sub + 1) * 128], h2T[:, kc, tk], kc == 0, kc == KC - 1, [wun], [pun],
                                     signal=(kc == KC - 1))
                            g_, gn = gt_[k2], f"mg{k2}"
                            s2, s2n = sg_[k2], f"msg{k2}"
                            u_, un = ut_[k2], f"mu{k2}"
                            P.ts("dve", g_[:], pg[:, :], bgu[:, e, fb:fb + 1], 7.0, ALU.add, ALU.min, [pgn, "bgu"], [gn])
                            P.act(s2[:], g_[:], AF.Sigmoid, [gn], [s2n], scale=1.702)
                            P.ts("dve", u_[:], pu[:, :], bgu[:, e, 16 + fb:17 + fb], 7.0, ALU.add, ALU.min, [pun, "bgu"], [un])
                            P.ts("dve", u_[:], u_[:], -7.0, 1.0, ALU.max, ALU.add, [un], [un])
                            P.tt("dve", g_[:], g_[:], s2[:], ALU.mult, [gn, s2n], [gn])
                            P.tt("dve", actT[:, fb, tk], u_[:], g_[:], ALU.mult, [un, gn], [f"actT{fb}"])
                for cb in range(8):
                    prefetch(kblk)
                    wd, wdnm = wb_(kblk)
                    kblk += 1
                    for i in range(NT):
                        pb, pn = ps[4 + i % 4], f"ps{4 + i % 4}"
                        for fb in range(KC):
                            P.mm(pb[:, 0:256], actT[:, fb, i * 128:(i + 1) * 128], wd[:, fb, :], fb == 0, fb == KC - 1,
                                 [wdnm, f"actT{fb}"], [pn], signal=(fb == KC - 1))
                        a_ = acc[:, i, cb * 256:(cb + 1) * 256]
                        P.stt(a_, pb[:, 0:256], gw[:, i, e:e + 1], a_, ALU.mult, ALU.add, [pn, f"acc{i}"], [f"acc{i}"])
            if "acc" in dbg:
                for i in range(NT):
                    P.dma("sp", dbg_tensor(f"acc{i}", [128, D]), acc[:, i, :], [f"acc{i}"], ())
            P.barrier()

        if "stop6" in dbg:
            return
        with contextlib.ExitStack() as st2:
            gtf = SB(st2, "gtf", [128, D]); fgb = SB(st2, "fgb", [128, D])
            xt = [SB(st2, f"x2t{i}", [128, D]) for i in range(2)]
            junk = SB(st2, "junk4", [128, D])
            ss = SB(st2, "ss3", [128, NT]); rstd = SB(st2, "rstd3", [128, NT])
            P.dma("sp", gtf[:], mod_d[0:1, 5 * D:6 * D].partition_broadcast(128), (), ["gtf"])
            P.dma("sp", fgb[:], fgd[0:1, :].partition_broadcast(128), (), ["fgb"])
            P.memset("pool", ss[:], 0.0, ["ss3"])
            for i in range(NT):
                x_, xn = xt[i % 2], f"x2t{i % 2}"
                P.dma("sp", x_[:], x1_d[i * 128:(i + 1) * 128, :], (), [xn])
                P.tt("pool", acc[:, i, :], acc[:, i, :], gtf[:], ALU.mult, ["gtf", f"acc{i}"], [f"acc{i}"])
                P.tt("dve", x_[:], x_[:], acc[:, i, :], ALU.add, [xn, f"acc{i}"], [xn])
                P.act(junk[:], x_[:], AF.Square, [xn], ["junk4", "ss3"], accum_out=ss[:, i:i + 1])
                P.rsqrt(rstd[:, i:i + 1], ss[:, i:i + 1], 1.0 / D, RMS_EPS, ["ss3"], ["rstd3"])
                P.stt(x_[:], x_[:], rstd[:, i:i + 1], fgb[:], ALU.mult, ALU.mult, [xn, "rstd3", "fgb"], [xn])
                P.dma("sp", out_d[i * 128:(i + 1) * 128, :], x_[:], [xn], ["out_d"])
            P.barrier()
    P.barrier()


def build(upto=99, dbg=()):
    nc = bass.Bass("TRN2", target_bir_lowering=False)
    P = Prog(nc)
    dbg_out = {}

    def din(name, shape, dt=F32):
        return nc.dram_tensor(name, list(shape), dt, kind="ExternalInput").ap()

    xb = din("xb", [T, D])
    c128 = din("c128", [128, KC])
    sel_d = din("sel", [128, 2])
    ada_wb = din("ada_wb", [24, 128, KC, 512])
    ada_b = din("ada_b", [1, 6 * D])
    n1g = din("n1g", [1, D])
    w_in_blk = din("w_in_blk", [NWB, 128, KC, 128])
    out_d = nc.dram_tensor("out", [NTOK, D], F32, kind="ExternalOutput").ap()
    mod_d = nc.dram_tensor("mod_d", [1, 6 * D], F32, kind="ExternalOutput" if "mod" in dbg else "Internal").ap()

    def dbg_tensor(name, shape, dt=F32):
        t = nc.dram_tensor("dbg_" + name, list(shape), dt, kind="ExternalOutput").ap()
        dbg_out[name] = t
        return t

    with contextlib.ExitStack() as st0:
        def SB(st, name, shape, dt=F32):
            return st.enter_context(nc.sbuf_tensor("sb_" + name, list(shape), dt))

        ps = [st0.enter_context(nc.psum_tensor(f"ps{i}", [128, 512], F32)) for i in range(8)]
        identb = SB(st0, "identb", [128, 128], BF16)
        identf = SB(st0, "identf", [128, 128], F32)
        ones1 = SB(st0, "ones1", [1, 128], F32)
        sel = SB(st0, "sel", [128, 2], F32)
        hT = SB(st0, "hT", [128, KC, T], BF16)

        P.memset("pool", identf[:], 0.0, ["identf"])
        P.op("pool", lambda e: e.affine_select(identf[:], identf[:], [[-1, 128]], ALU.not_equal, 1.0,
                                               base=0, channel_multiplier=1), ["identf"], ["identf"])
        P.cp("dve", identb[:], identf[:], ["identf"], ["identb"])
        P.memset("pool", ones1[:], 1.0, ["ones1"])
        P.dma("sp", sel[:], sel_d, (), ["sel"])

        with contextlib.ExitStack() as st:
            c_sb = SB(st, "c_sb", [128, KC])
            mrow = [SB(st, f"mrow{i}", [1, 512]) for i in range(2)]
            brow = [SB(st, f"brow{i}", [1, 512]) for i in range(2)]
            scb = SB(st, "scb", [128, KC], BF16)
            wst = [SB(st, f"adaw{i}", [128, KC, 512], BF16) for i in range(3)]
            P.dma("sp", c_sb[:], c128, (), ["c_sb"])
            P.act(scb[:], c_sb[:], AF.Silu, ["c_sb"], ["scb"])
            for nb in range(24):
                w = wst[nb % 3]
                wn = f"adaw{nb % 3}"
                P.dma("pool", w[:], ada_wb[nb], (), [wn])
                P.dma("sp", brow[nb % 2][:], ada_b[0:1, nb * 512:(nb + 1) * 512], (), [f"brow{nb % 2}"])
                pb = ps[nb % 2]
                pn = f"ps{nb % 2}"
                for kc in range(KC):
                    P.mm(pb[0:1, :], scb[:, kc:kc + 1], w[:, kc, :], kc == 0, kc == KC - 1, ["scb", wn], [pn], signal=(kc == KC - 1))
                P.tt("dve", mrow[nb % 2][0:1, :], pb[0:1, :], brow[nb % 2][0:1, :], ALU.add, [pn, f"brow{nb % 2}"], [f"mrow{nb % 2}"])
                P.dma("sp", mod_d[0:1, nb * 512:(nb + 1) * 512], mrow[nb % 2][:], [f"mrow{nb % 2}"], ["mod_d"])
            P.barrier()

        with contextlib.ExitStack() as st:
            g1s = SB(st, "g1s", [128, D])
            shm = SB(st, "shm", [128, D])
            n1b = SB(st, "n1b", [128, D])
            xt = [SB(st, f"xt{i}", [128, D]) for i in range(2)]
            hb = [SB(st, f"hb{i}", [128, D], BF16) for i in range(2)]
            junk = SB(st, "junk", [128, D])
            ss = SB(st, "ss", [128, 16])
            rstd = SB(st, "rstd", [128, 16])
            P.memset("pool", ss[:], 0.0, ["ss"])
            P.dma("sp", g1s[:], mod_d[0:1, D:2 * D].partition_broadcast(128), ["mod_d"], ["g1s"])
            P.dma("sp", shm[:], mod_d[0:1, 0:D].partition_broadcast(128), ["mod_d"], ["shm"])
            P.dma("sp", n1b[:], n1g[0:1, :].partition_broadcast(128), (), ["n1b"])
            P.stt(g1s[:], g1s[:], 1.0, n1b[:], ALU.add, ALU.mult, ["g1s", "n1b"], ["g1s"])
            for i in range(16):
                x_ = xt[i % 2]
                xn = f"xt{i % 2}"
                h_ = hb[i % 2]
                hn = f"hb{i % 2}"
                P.dma("sp", x_[:], xb[i * 128:(i + 1) * 128, :], (), [xn])
                P.act(junk[:], x_[:], AF.Square, [xn], ["junk", "ss"], accum_out=ss[:, i:i + 1])
                P.rsqrt(rstd[:, i:i + 1], ss[:, i:i + 1], 1.0 / D, RMS_EPS, ["ss"], ["rstd"])
                P.stt(x_[:], x_[:], rstd[:, i:i + 1], g1s[:], ALU.mult, ALU.mult, [xn, "rstd", "g1s"], [xn])
                P.tt("dve", h_[:], x_[:], shm[:], ALU.add, [xn, "shm"], [hn])
                for half in range(2):
                    pb = ps[(2 * i + half) % 4]
                    pn = f"ps{(2 * i + half) % 4}"
                    pbb = pb[:].bitcast(BF16)
                    for q in range(8):
                        kc = half * 8 + q
                        P.tr(pbb[:, q * 128:(q + 1) * 128], h_[:, kc * 128:(kc + 1) * 128], identb[:],
                             [hn, "identb"], [pn], signal=(q == 7))
                    P.cp("act" if half else "dve", hT[:, half * 8:(half + 1) * 8, i * 128:(i + 1) * 128],
                         pbb[:, 0:1024].rearrange("p (k t) -> p k t", k=8), [pn], [f"hT{i}"])
            if "hT" in dbg:
                P.dma("sp", dbg_tensor("hT", [128, KC, T], BF16), hT[:], [f"hT{i}" for i in range(16)], ())
            P.barrier()

        yT = SB(st0, "yT", [128, KC, NTOK], BF16)
        blockones = SB(st0, "blockones", [128, 128])
        P.memset("pool", blockones[:], 0.0, ["blockones"])
        P.memset("pool", blockones[0:64, 0:64], 1.0, ["blockones"])
        P.memset("pool", blockones[64:128, 64:128], 1.0, ["blockones"])

        if upto >= 3 and "skip_rwkv" not in dbg:
            rwkv_phase(nc, P, SB, ps, hT, yT, identf, identb, blockones, sel, w_in_blk, din, dbg, dbg_tensor)
        if upto >= 4 and "skip_mlstm" not in dbg:
            mlstm_phase(nc, P, SB, ps, hT, yT, identf, identb, sel, w_in_blk, din, dbg, dbg_tensor)
        if "yT" in dbg:
            P.dma("sp", dbg_tensor("yT", [128, KC, NTOK], BF16), yT[:], ["yT"], ())
            P.barrier()
        if upto >= 5:
            ffn_phase(nc, P, SB, ps, hT, yT, identf, identb, din, mod_d, out_d, dbg, dbg_tensor)

        P.barrier()
        P.emit()
    return nc, dbg_out


def prep_inputs(inputs):
    f = lambda a: np.ascontiguousarray(a, dtype=np.float32)
    shared = {}
    ada_w = inputs["ada_w"][0]
    shared["ada_wb"] = f(ada_w.reshape(KC, 128, 24, 512).transpose(2, 1, 0, 3))
    shared["ada_b"] = f(inputs["ada_b"])
    shared["n1g"] = f(inputs["norm1_g"])
    w_in = inputs["w_in"][0]
    idx = w_in_col_index()
    wpad = np.concatenate([w_in, np.zeros((D, 1), np.float32)], axis=1)
    blk = wpad[:, np.where(idx < 0, w_in.shape[1], idx)]
    shared["w_in_blk"] = f(blk.reshape(KC, 128, NWB, 128).transpose(2, 1, 0, 3))
    mu = inputs["rwkv_mu"][0]
    chs = lambda p: np.concatenate([64 * p + np.arange(64), 64 * (p + 8) + np.arange(64)])
    vecs = [mu[0:1024], mu[1088:2112], mu[2112:3136], inputs["rwkv_w0"][0], inputs["rwkv_a0"][0], inputs["rwkv_kk"][0],
            inputs["rwkv_ka"][0], inputs["rwkv_rk"][0].reshape(1024), inputs["rwkv_ln_w"][0], inputs["rwkv_ln_b"][0]]
    shared["rw_cols"] = f(np.stack([np.stack([v[chs(p)] for v in vecs], -1) for p in range(8)], 1))
    lcols = np.zeros((128, 3), np.float32)
    lcols[:64, 0] = mu[1024:1088]
    lcols[64:, 0] = mu[3136:3200]
    lcols[:, 1] = mu[3200:3328]
    lcols[:32, 2] = mu[3328:3360]
    shared["lora_cols"] = lcols
    w2, a2, g2 = inputs["rwkv_w2"][0], inputs["rwkv_a2"][0], inputs["rwkv_g2"][0]
    shared["wa2"] = f(np.stack([np.concatenate([w2[:, chs(p)], a2[:, chs(p)]], 0) for p in range(8)], 1))
    shared["g2a"] = f(np.stack([g2[0:128][:, chs(p)] for p in range(8)], 1))
    shared["g2b"] = f(np.stack([g2[128:160][:, chs(p)] for p in range(8)], 1))
    cw, cb = inputs["mlstm_conv_w"][0], inputs["mlstm_conv_b"][0]
    mlc = np.zeros((128, 16, 5), np.float32)
    for i in range(16):
        mlc[:, i, 0:4] = cw[:, i * 128:(i + 1) * 128].T
        mlc[:, i, 4] = cb[i * 128:(i + 1) * 128]
    shared["ml_cols"] = mlc
    shared["ml_gb"] = f(np.stack([inputs["mlstm_b_i"][0], inputs["mlstm_b_f"][0]], 1))
    shared["ml_ng"] = f(inputs["mlstm_norm_g"])
    chs_all = np.concatenate([chs(p) for p in range(8)] + [1024 + np.arange(1024)])
    shared["w_out_blk"] = f(inputs["w_out"][0][chs_all].reshape(KC, 128, 4, 512).transpose(2, 1, 0, 3))
    shared["n2g"] = f(inputs["norm2_g"])
    shared["fg"] = f(inputs["final_g"].reshape(1, D))
    shared["router_w_r"] = f(inputs["router_w"][0].reshape(KC, 128, NE).transpose(1, 0, 2))
    shared["router_b"] = f(inputs["router_b"])
    shared["bgu_cols"] = f(inputs["moe_b_gu"][0].reshape(NE, 32, 128).transpose(2, 0, 1))
    shared["b_dn"] = f(inputs["moe_b_dn"][0])
    if "moe_w_gu" in inputs:
        g_ = inputs["moe_w_gu"][0]
        ne = g_.shape[0]
        shared["moe_w_gu"] = f(g_.reshape(ne, KC, 128, 16, 256).transpose(0, 3, 2, 1, 4))
        d_ = inputs["moe_w_dn"][0]
        shared["moe_w_dn"] = f(d_.reshape(ne, KC, 128, 8, 256).transpose(0, 3, 2, 1, 4))
    per_core = []
    for core in range(8):
        b, j = core // 2, core % 2
        d = dict(shared)
        d["xb"] = f(inputs["x"][b])
        d["xm"] = f(inputs["x"][b, j * NTOK:(j + 1) * NTOK])
        d["c128"] = f(inputs["c"][b].reshape(KC, 128).T)
        s = np.zeros((128, 2), np.float32)
        s[:, j] = 1.0
        d["sel"] = s
        per_core.append(d)
    return per_core


def kernel(**inputs):
    nc, _ = build()
    in_maps = prep_inputs(inputs)
    res = run_bass_kernel_spmd(nc, in_maps, core_ids=list(range(8)))
    out = np.zeros((4, T, D), np.float32)
    for core in range(8):
        b, j = core // 2, core % 2
        out[b, j * NTOK:(j + 1) * NTOK] = res.results[core]["out"]
    return out
```
